# Optimizing a Trainium2 kernel written in Bass

```python
import math
import jax, jax.numpy as jnp
from jax import lax
import numpy as np

D_MODEL = 1024
BATCH = 16
SEQ = 4096
DEPTH = 1

D_RNN = 1024
N_RNN_BLOCKS = 16
RNN_BLOCK = D_RNN // N_RNN_BLOCKS
CONV_WIDTH = 4
LRU_C = 8.0
N_HEADS = 16
N_KV_HEADS = 4
HEAD_DIM = 64
ROT_DIM = HEAD_DIM // 4
ROPE_THETA = 500000.0
N_IDX_HEADS = 8
IDX_DIM = 64
IDX_ROT_DIM = IDX_DIM // 4
TOPK_MAX = 256
Q_BLOCK = 128
D_FF = -(-8 * D_MODEL // (3 * 256)) * 256
EPS = 1e-6

IN_SPLITS = (D_RNN, D_RNN, N_HEADS * HEAD_DIM, N_KV_HEADS * HEAD_DIM, N_KV_HEADS * HEAD_DIM,
             N_IDX_HEADS * IDX_DIM, IDX_DIM, N_IDX_HEADS, D_MODEL, D_MODEL)
D_IN = sum(IN_SPLITS)

kernel_name = "hybrid_rglru_dsa_gated_block"


def rms_norm(x, g):
    x32 = x.astype(jnp.float32)
    y = x32 * lax.rsqrt(jnp.mean(x32 * x32, axis=-1, keepdims=True) + EPS)
    return (y * g.astype(jnp.float32)).astype(x.dtype)


def layer_norm(x, g, b):
    x32 = x.astype(jnp.float32)
    mu = jnp.mean(x32, axis=-1, keepdims=True)
    var = jnp.mean(jnp.square(x32 - mu), axis=-1, keepdims=True)
    y = (x32 - mu) * lax.rsqrt(var + EPS)
    return (y * g.astype(jnp.float32) + b.astype(jnp.float32)).astype(x.dtype)


def partial_rope(x, rot_dim):
    s = x.shape[1]
    half = rot_dim // 2
    inv_freq = ROPE_THETA ** (-jnp.arange(half, dtype=jnp.float32) / half)
    ang = jnp.arange(s, dtype=jnp.float32)[:, None] * inv_freq[None, :]
    cos = jnp.cos(ang)[None, :, None, :]
    sin = jnp.sin(ang)[None, :, None, :]
    xr = x[..., :rot_dim].astype(jnp.float32)
    x1, x2 = xr[..., :half], xr[..., half:]
    rot = jnp.concatenate([x1 * cos - x2 * sin, x2 * cos + x1 * sin], axis=-1).astype(x.dtype)
    return jnp.concatenate([rot, x[..., rot_dim:]], axis=-1)


def causal_depthwise_conv(x, w, b):
    s = x.shape[1]
    xp = jnp.pad(x, ((0, 0), (CONV_WIDTH - 1, 0), (0, 0)))
    out = b
    for k in range(CONV_WIDTH):
        out = out + xp[:, k:k + s] * w[k]
    return out


def rg_lru(x, w_a, b_a, w_x, b_x, lam):
    bsz, s, _ = x.shape
    xb = x.reshape(bsz, s, N_RNN_BLOCKS, RNN_BLOCK)
    r = jax.nn.sigmoid(jnp.einsum('bshi,hij->bshj', xb, w_a).reshape(bsz, s, D_RNN) + b_a)
    i = jax.nn.sigmoid(jnp.einsum('bshi,hij->bshj', xb, w_x).reshape(bsz, s, D_RNN) + b_x)
    log_a = (-LRU_C * r.astype(jnp.float32)) * jax.nn.softplus(-lam.astype(jnp.float32))
    a = jnp.exp(log_a)
    u = jnp.sqrt(-jnp.expm1(2.0 * log_a)) * (i * x).astype(jnp.float32)

    def combine(left, right):
        a_l, b_l = left
        a_r, b_r = right
        return a_r * a_l, a_r * b_l + b_r

    _, h = lax.associative_scan(combine, (a, u), axis=1)
    return h.astype(x.dtype)


def dsa_sparse_attention(q, k, v, iq, ik, iw):
    bsz, s = q.shape[0], q.shape[1]
    topk = min(TOPK_MAX, s // 4)
    n_blk = s // Q_BLOCK
    rep = N_HEADS // N_KV_HEADS
    key_pos = jnp.arange(s)
    gather = jax.vmap(lambda t, idx: t[idx])

    def to_blocks(t):
        return jnp.moveaxis(t.reshape(bsz, n_blk, Q_BLOCK, *t.shape[2:]), 1, 0)

    def one_block(args):
        blk, qb, iqb, iwb = args
        q_pos = blk * Q_BLOCK + jnp.arange(Q_BLOCK)
        causal = key_pos[None, :] <= q_pos[:, None]
        logits = jnp.einsum('bqhd,bsd->bqhs', iqb, ik, preferred_element_type=jnp.float32)
        score = jnp.einsum('bqhs,bqh->bqs', jax.nn.relu(logits), iwb.astype(jnp.float32))
        score = jnp.where(causal[None], score, -jnp.inf)
        _, idx = lax.top_k(score, topk)
        valid = idx <= q_pos[None, :, None]
        k_sel = gather(k, idx)
        v_sel = gather(v, idx)
        qg = qb.reshape(bsz, Q_BLOCK, N_KV_HEADS, rep, HEAD_DIM)
        att = jnp.einsum('bqgrd,bqkgd->bqgrk', qg, k_sel,
                         preferred_element_type=jnp.float32) * (HEAD_DIM ** -0.5)
        att = jnp.where(valid[:, :, None, None, :], att, -jnp.inf)
        p = jax.nn.softmax(att, axis=-1).astype(v.dtype)
        o = jnp.einsum('bqgrk,bqkgd->bqgrd', p, v_sel)
        return o.reshape(bsz, Q_BLOCK, N_HEADS * HEAD_DIM)

    out = lax.map(one_block, (jnp.arange(n_blk), to_blocks(q), to_blocks(iq), to_blocks(iw)))
    return jnp.moveaxis(out, 0, 1).reshape(bsz, s, N_HEADS * HEAD_DIM)


def setup_inputs(seed: int = 0) -> dict:
    key = jax.random.key(seed)
    ks = jax.random.split(key, 24)
    f32 = jnp.float32

    def nrm(k, shape, fan_in):
        return jax.random.normal(k, shape, f32) * (fan_in ** -0.5)

    def gain(k, shape):
        return 1.0 + 0.05 * jax.random.normal(k, shape, f32)

    L = DEPTH
    u = jax.random.uniform(ks[9], (L, D_RNN), f32, 0.9, 0.999)
    s_lam = u ** (1.0 / LRU_C)
    rg_lambda = jnp.log(s_lam) - jnp.log1p(-s_lam)
    return {
        "x": jax.random.normal(ks[0], (BATCH, SEQ, D_MODEL), f32),
        "norm_mix_pre": gain(ks[1], (L, D_MODEL)),
        "w_in": nrm(ks[2], (L, D_MODEL, D_IN), D_MODEL),
        "conv_w": nrm(ks[3], (L, CONV_WIDTH, D_RNN), CONV_WIDTH),
        "conv_b": 0.02 * jax.random.normal(ks[4], (L, D_RNN), f32),
        "rg_w_a": nrm(ks[5], (L, N_RNN_BLOCKS, RNN_BLOCK, RNN_BLOCK), RNN_BLOCK),
        "rg_b_a": 0.02 * jax.random.normal(ks[6], (L, D_RNN), f32),
        "rg_w_x": nrm(ks[7], (L, N_RNN_BLOCKS, RNN_BLOCK, RNN_BLOCK), RNN_BLOCK),
        "rg_b_x": 0.02 * jax.random.normal(ks[8], (L, D_RNN), f32),
        "rg_lambda": rg_lambda,
        "idx_k_ln_g": gain(ks[10], (L, IDX_DIM)),
        "idx_k_ln_b": 0.02 * jax.random.normal(ks[11], (L, IDX_DIM), f32),
        "w_rnn_out": nrm(ks[12], (L, D_RNN, D_MODEL), D_RNN),
        "w_att_out": nrm(ks[13], (L, N_HEADS * HEAD_DIM, D_MODEL), N_HEADS * HEAD_DIM),
        "w_o": nrm(ks[14], (L, D_MODEL, D_MODEL), D_MODEL),
        "norm_mix_post": gain(ks[15], (L, D_MODEL)),
        "norm_ffn_pre": gain(ks[16], (L, D_MODEL)),
        "w_ffn_gate": nrm(ks[17], (L, D_MODEL, D_FF), D_MODEL),
        "w_ffn_up": nrm(ks[18], (L, D_MODEL, D_FF), D_MODEL),
        "w_ffn_down": nrm(ks[19], (L, D_FF, D_MODEL), D_FF),
        "norm_ffn_post": gain(ks[20], (L, D_MODEL)),
    }


def reference(x, norm_mix_pre, w_in, conv_w, conv_b, rg_w_a, rg_b_a, rg_w_x, rg_b_x, rg_lambda,
              idx_k_ln_g, idx_k_ln_b, w_rnn_out, w_att_out, w_o, norm_mix_post,
              norm_ffn_pre, w_ffn_gate, w_ffn_up, w_ffn_down, norm_ffn_post):
    bsz, s, _ = x.shape
    idx_w_scale = (N_IDX_HEADS ** -0.5) * (IDX_DIM ** -0.5)
    for l in range(DEPTH):
        h = rms_norm(x, norm_mix_pre[l])
        proj = h @ w_in[l]
        parts = []
        start = 0
        for width in IN_SPLITS:
            parts.append(proj[..., start:start + width])
            start += width
        xr, gr, q, k, v, iq, ik, iw, g_a, g_b = parts

        xc = causal_depthwise_conv(xr, conv_w[l], conv_b[l])
        y_rnn = rg_lru(xc, rg_w_a[l], rg_b_a[l], rg_w_x[l], rg_b_x[l], rg_lambda[l]) * jax.nn.gelu(gr)
        y_a = y_rnn @ w_rnn_out[l]

        q = partial_rope(q.reshape(bsz, s, N_HEADS, HEAD_DIM), ROT_DIM)
        k = partial_rope(k.reshape(bsz, s, N_KV_HEADS, HEAD_DIM), ROT_DIM)
        v = v.reshape(bsz, s, N_KV_HEADS, HEAD_DIM)
        iq = partial_rope(iq.reshape(bsz, s, N_IDX_HEADS, IDX_DIM), IDX_ROT_DIM)
        ik = layer_norm(ik, idx_k_ln_g[l], idx_k_ln_b[l])
        ik = partial_rope(ik[:, :, None, :], IDX_ROT_DIM)[:, :, 0, :]
        iw = iw * idx_w_scale
        y_att = dsa_sparse_attention(q, k, v, iq, ik, iw)
        y_b = y_att @ w_att_out[l]

        merged = jax.nn.sigmoid(g_a) * y_a + jax.nn.sigmoid(g_b) * y_b
        mix = merged @ w_o[l]
        x = x + rms_norm(mix, norm_mix_post[l])

        h = rms_norm(x, norm_ffn_pre[l])
        f = (jax.nn.silu(h @ w_ffn_gate[l]) * (h @ w_ffn_up[l])) @ w_ffn_down[l]
        x = x + rms_norm(f, norm_ffn_post[l])
    return x
```

```python
import contextlib
import math
import numpy as np
import concourse.bass as bass
import concourse.mybir as mybir
from concourse.bass_utils import run_bass_kernel_spmd

F32 = mybir.dt.float32
BF16 = mybir.dt.bfloat16
AF = mybir.ActivationFunctionType
ALU = mybir.AluOpType
AX = mybir.AxisListType

D = 1024
KC = 8
T = 512
NJ = 4
DFF = 2816
FC = 22
DIN = 6216
EPS = 1e-6
NEG_FILL = -1.0e30
NEG_THR = -1.0e29
MASK_BIG = -30000.0


class Sched:
    ENG = ("pe", "act", "dve", "pool", "sp")

    def __init__(self, nc, stack):
        self.nc = nc
        self.stack = stack
        self.e = {"pe": nc.tensor, "act": nc.scalar, "dve": nc.vector, "pool": nc.gpsimd, "sp": nc.sync}
        self.psem = {}
        self.cnt = {}
        for n in ("pe", "act", "dve", "pool"):
            self.psem[n] = stack.enter_context(nc.semaphore("prog_" + n))
            self.cnt[n] = 0
        self.known = {n: {} for n in self.ENG}
        self.lastw = {}
        self.readers = {}
        self.dsem = {}
        self.dcnt = {}
        self.slot_keys = {}
        self.nwaits = 0
        self.ninstr = 0

    def _deps(self, eng, reads, writes):
        deps = {}

        def add(p):
            s, v = p[0], p[1]
            if id(s) not in deps or deps[id(s)][1] < v:
                deps[id(s)] = (s, v)

        for k in reads:
            p = self.lastw.get(k)
            if p is not None and not (p[2] == eng and eng == "pe"):
                add(p)
        for k in writes:
            p = self.lastw.get(k)
            if p is not None and not (p[2] == eng and eng == "pe"):
                add(p)
            for p in self.readers.get(k, {}).values():
                if not (p[2] == eng and eng == "pe"):
                    add(p)
        return deps

    def _emit_waits(self, eng, deps):
        kn = self.known[eng]
        for s, v in deps.values():
            if kn.get(id(s), 0) >= v:
                continue
            self.e[eng].wait_ge(s, v)
            kn[id(s)] = v
            self.nwaits += 1

    def _record(self, reads, writes, token):
        for k in reads:
            self.readers.setdefault(k, {})[id(token[0])] = token
        for k in writes:
            self.lastw[k] = token
            self.readers[k] = {}

    def op(self, eng, fn, reads=(), writes=()):
        deps = self._deps(eng, reads, writes)
        self._emit_waits(eng, deps)
        ins = fn()
        self.cnt[eng] += 1
        ins.then_inc(self.psem[eng], 1)
        self._record(reads, writes, (self.psem[eng], self.cnt[eng], eng))
        self.ninstr += 1
        return ins

    def dma(self, queue, out, in_, slot, reads=(), writes=(), **kw):
        if slot not in self.dsem:
            self.dsem[slot] = self.stack.enter_context(self.nc.semaphore("d_" + str(slot)))
            self.dcnt[slot] = 0
        deps = self._deps(queue, reads, writes)
        self._emit_waits(queue, deps)
        ins = self.e[queue].dma_start(out=out, in_=in_, **kw)
        self.dcnt[slot] += 16
        ins.then_inc(self.dsem[slot], 16)
        self._record(reads, writes, (self.dsem[slot], self.dcnt[slot], "dma"))
        self.slot_keys.setdefault(slot, set()).update(writes)
        self.ninstr += 1
        return ins

    def seal(self, slot):
        for k in self.slot_keys.get(slot, ()):
            self.lastw[k] = (self.dsem[slot], self.dcnt[slot], "dma")

    def wait_keys(self, eng, keys):
        deps = self._deps(eng, keys, ())
        self._emit_waits(eng, deps)


class _Stop(Exception):
    pass


def build_program(NB, S, NITER=16, TOPK=256, debug=None):
    NT = S // T
    NBLK = S // 128
    nc = bass.Bass("TRN2", target_bir_lowering=False, dynamic_dma_scratch_size=4096)
    dt = lambda name, shape, dtype=F32, kind="ExternalInput": nc.dram_tensor(name, shape, dtype, kind=kind).ap()
    x = dt("x", [NB, S, D])
    out = dt("out", [NB, S, D], kind="ExternalOutput")
    norm_mix_pre = dt("norm_mix_pre", [1, D])
    w_in = dt("w_in", [1, D, DIN])
    conv_w = dt("conv_w", [1, 4, D])
    conv_b = dt("conv_b", [1, D])
    rg_w_a = dt("rg_w_a", [1, 16, 64, 64])
    rg_b_a = dt("rg_b_a", [1, D])
    rg_w_x = dt("rg_w_x", [1, 16, 64, 64])
    rg_b_x = dt("rg_b_x", [1, D])
    rg_lambda = dt("rg_lambda", [1, D])
    idx_k_ln_g = dt("idx_k_ln_g", [1, 64])
    idx_k_ln_b = dt("idx_k_ln_b", [1, 64])
    w_rnn_out = dt("w_rnn_out", [1, D, D])
    w_att_out = dt("w_att_out", [1, D, D])
    w_o = dt("w_o", [1, D, D])
    norm_mix_post = dt("norm_mix_post", [1, D])
    norm_ffn_pre = dt("norm_ffn_pre", [1, D])
    w_ffn_gate = dt("w_ffn_gate", [1, D, DFF])
    w_ffn_up = dt("w_ffn_up", [1, D, DFF])
    w_ffn_down = dt("w_ffn_down", [1, DFF, D])
    norm_ffn_post = dt("norm_ffn_post", [1, D])
    rope_tab = dt("rope_tab", [S, 32])
    ident_in = dt("ident_in", [128, 128])
    pow2_in = dt("pow2_in", [128, 64])

    Wfm = dt("Wfm", [D, 4096], BF16, kind="Internal")
    Wtk = dt("Wtk", [D, 2120], BF16, kind="Internal")
    Wrnn = dt("Wrnn", [D, D], BF16, kind="Internal")
    Watt = dt("Watt", [D, D], BF16, kind="Internal")
    Wo = dt("Wo", [D, D], BF16, kind="Internal")
    Wg = dt("Wg", [D, DFF], BF16, kind="Internal")
    Wu = dt("Wu", [D, DFF], BF16, kind="Internal")
    Wd = dt("Wd", [DFF, D], BF16, kind="Internal")

    dbg = dt("dbg", [128, 4096], kind="ExternalOutput") if debug else None
    st = contextlib.ExitStack()
    with st:
        Sx = Sched(nc, st)

        def ck(name, dumps=()):
            if debug != name:
                return
            col = 0
            for di, (ap, key) in enumerate(dumps):
                n = ap.shape[-1]
                if ap.dtype == BF16:
                    stg = st.enter_context(nc.sbuf_tensor("dbgst%d" % di, [128, n], F32))
                    OP("act", lambda: nc.scalar.copy(out=stg[0:ap.shape[0], :], in_=ap), reads=key if isinstance(key, list) else [key], writes=[("dbgst", di)])
                    Sx.dma("sp", dbg[0:ap.shape[0], col:col + n], stg[0:ap.shape[0], :], "dbg", reads=[("dbgst", di)])
                else:
                    Sx.dma("sp", dbg[0:ap.shape[0], col:col + n], ap, "dbg", reads=key if isinstance(key, list) else [key])
                col += n
            raise _Stop()

        sb = lambda n, s, d=F32: st.enter_context(nc.sbuf_tensor(n, s, d))
        OP = Sx.op

        x_sb = sb("x_sb", [128, NJ, D])
        score = x_sb[:].rearrange("p j d -> p (j d)")
        xn = sb("xn", [128, NJ, D], BF16)
        nm = xn[:].rearrange("p j d -> p (j d)")
        score2 = sb("score2", [128, 4096])
        nm2 = sb("nm2", [128, 4096], BF16)
        scoreb = [score, score2]
        nmb = [nm, nm2]
        mergedT = xn[:].rearrange("p j d -> p (j d)").rearrange("p (k t) -> p k t", k=KC)
        hT = sb("hT", [128, KC, T], BF16)
        actT = sb("actT", [128, FC, T], BF16)
        yrT = actT[:, 0:8, :]
        yattT = actT[:, 8:16, :]
        NWB = 4
        wbuf = [sb("wbuf%d" % i, [128, 4096], BF16) for i in range(NWB)]
        wstage = wbuf[0][:].bitcast(F32).rearrange("p (w c j) -> p w c j", w=2, c=KC)
        KT2 = sb("KT2", [128, 2, S], BF16)
        ikT2 = sb("ikT2", [128, S], BF16)
        Vp = sb("Vp", [128, NBLK, 4, 65], BF16)
        qz = sb("qz", [128, 16, T], BF16)
        iqz = sb("iqz", [128, 8, T], BF16)
        absw = sb("absw", [128, NJ, 8])
        sgnw = sb("sgnw", [128, NJ, 8])
        ident = sb("ident", [128, 128], BF16)
        ident4 = sb("ident4", [128, 4, 128], BF16)
        identf = sb("identf", [128, 128])
        g1 = sb("g1", [128, KC])
        g3 = sb("g3", [128, KC])
        g2b = sb("g2b", [128, D])
        g4b = sb("g4b", [128, D])
        lngb = sb("lngb", [128, 2, 64])
        cw = sb("cw", [128, 4, KC])
        cb = sb("cb", [128, KC])
        ba = sb("ba", [128, KC])
        bx = sb("bx", [128, KC])
        lam = sb("lam", [128, KC])
        cneg = sb("cneg", [128, KC])
        cneg2 = sb("cneg2", [128, KC])
        Wa_bd = sb("Wa_bd", [128, KC, 128], BF16)
        Wx_bd = sb("Wx_bd", [128, KC, 128], BF16)
        rope = sb("rope", [128, NBLK, 32])
        pow2 = sb("pow2", [128, 64])
        epsT = sb("epsT", [128, 1])
        oneT = sb("oneT", [128, 1])
        halo = sb("halo", [128, KC, 3])
        hstate = sb("hstate", [128, KC])
        xrh = sb("xrh", [128, 515])
        xc = sb("xc", [128, 512])
        xcb = sb("xcb", [128, 512], BF16)
        qkb = sb("qkb", [128, 512], BF16)
        rtmp = sb("rtmp", [128, 8, 16])
        rtmp2 = sb("rtmp2", [128, 8, 16])
        rh = [sb("rh%d" % i, [128, 512], BF16) for i in range(2)]
        dsg = sb("dsg", [128, 8, 128], BF16)
        pT = [sb("pT%d" % i, [128, 512], BF16) for i in range(3)]
        yatt = sb("yatt", [128, D], BF16)
        sg = [sb("sg%d" % i, [128, 512]) for i in range(2)]
        m1 = [sb("m1_%d" % i, [128, 512]) for i in range(2)]
        tokp = sg
        actflat = actT[:].rearrange("p f t -> p (f t)")
        rr = actflat[:, 16 * T:18 * T].bitcast(F32)
        ii = actflat[:, 18 * T:20 * T].bitcast(F32)
        ptmp = actflat[:, 20 * T:22 * T].bitcast(F32)
        aa, ss_, hh, gl = sg[0], sg[1], m1[0], m1[1]
        stat = sb("stat", [128, 16])
        bnst = sb("bnst", [128, 6])
        bnag = sb("bnag", [128, 2])
        lntmp = sb("lntmp", [128, 64])
        bis = sb("bis", [128, 8])
        wk = sb("wk", [128, 64])
        rec = sb("rec", [128, 4])
        asum = sb("asum", [128, 16])
        arec = sb("arec", [128, 16])

        ps = [st.enter_context(nc.psum_tensor("ps%d" % i, [128, 512], F32)) for i in range(8)]
        psb = [p[:].bitcast(BF16) for p in ps]

        rr_ctr = {"mm": 0, "tp": 0, "tok": 0, "rh": 0, "pT": 0, "sg": 0, "po": 0, "idx": 0, "pa": 0, "acc": 0}

        def bkeys(bk):
            return [("ps", bk)] if bk < 6 else [("pst", bk)]

        def next_bank(pool="mm", n=6):
            i = rr_ctr[pool] % n
            rr_ctr[pool] += 1
            return i
        next_bank_ = next_bank

        cq = 0
        def pdma(out_ap, in_ap, wkeys, **kw):
            nonlocal cq
            cq += 1
            Sx.dma("pool", out_ap, in_ap, "prep", writes=[], **kw)

        def sdma(out_ap, in_ap, wkeys, **kw):
            nonlocal cq
            cq += 1
            Sx.dma("sp", out_ap, in_ap, "cst%d" % cq, writes=wkeys, **kw)

        sdma(identf[:], ident_in, ["identf"])
        sdma(pow2[:], pow2_in, ["pow2"])
        sdma(rope[:], rope_tab.rearrange("(b p) c -> p b c", p=128), ["rope"])
        sdma(g2b[:], norm_mix_post[0].partition_broadcast(128), ["g2b"])
        sdma(g4b[:], norm_ffn_post[0].partition_broadcast(128), ["g4b"])
        sdma(lngb[:, 0, :], idx_k_ln_g[0].partition_broadcast(128), ["lng"])
        sdma(lngb[:, 1, :], idx_k_ln_b[0].partition_broadcast(128), ["lnb"])
        for tile_, src, k_ in ((g1, norm_mix_pre, "g1"), (g3, norm_ffn_pre, "g3"), (cb, conv_b, "cb"), (ba, rg_b_a, "ba"),
                               (bx, rg_b_x, "bx"), (lam, rg_lambda, "lam")):
            sdma(tile_[:], src[0].rearrange("(c p) -> p c", p=128), [k_], allow_slow_non_contiguous=True)
        sdma(cw[:], conv_w[0].rearrange("k (c p) -> p k c", p=128), ["cw"], allow_slow_non_contiguous=True)
        for wi, (wsrc, wdst, nm_) in enumerate(((rg_w_a, Wa_bd, "Wa_bd"), (rg_w_x, Wx_bd, "Wx_bd"))):
            OP("pool", lambda wdst=wdst: nc.gpsimd.memset(wdst[:], 0.0), writes=[nm_])
            v = wsrc[0].rearrange("(c two) i j -> two i c j", two=2)
            for two in range(2):
                Sx.dma("sp", wstage[two * 64:(two + 1) * 64, wi, :, 0:64], v[two], "wst%d%d" % (wi, two), writes=[("wstage", wi, two)])
                OP("act", lambda two=two, wi=wi, wdst=wdst: nc.scalar.activation(
                    out=wdst[two * 64:(two + 1) * 64, :, two * 64:(two + 1) * 64],
                    in_=wstage[two * 64:(two + 1) * 64, wi, :, 0:64], func=AF.Identity),
                   reads=[("wstage", wi, two)], writes=[nm_])
        OP("pool", lambda: nc.gpsimd.memset(Vp[:, :, :, 64:65], 1.0), writes=["Vp1"])
        OP("pool", lambda: nc.gpsimd.memset(qz[:], 0.0), writes=["qz0"])
        OP("pool", lambda: nc.gpsimd.memset(iqz[:], 0.0), writes=["iqz0"])
        OP("dve", lambda: nc.vector.memset(epsT[:], EPS), writes=["epsT"])
        OP("dve", lambda: nc.vector.memset(oneT[:], 1.0), writes=["oneT"])
        OP("dve", lambda: nc.vector.tensor_copy(out=ident[:], in_=identf[:]), reads=["identf"], writes=["ident"])
        for r in range(4):
            OP("dve", lambda r=r: nc.vector.tensor_copy(out=ident4[:, r, :], in_=identf[:]), reads=["identf"], writes=["ident4"])
        OP("act", lambda: nc.scalar.activation(out=cneg[:], in_=lam[:], func=AF.Exp, scale=-1.0), reads=["lam"], writes=["cneg"])
        OP("act", lambda: nc.scalar.activation(out=cneg[:], in_=cneg[:], func=AF.Ln, bias=oneT[:, 0:1], scale=1.0),
           reads=["cneg", "oneT"], writes=["cneg"])
        OP("dve", lambda: nc.vector.tensor_scalar(out=cneg2[:], in0=cneg[:], scalar1=-16.0, scalar2=None, op0=ALU.mult),
           reads=["cneg"], writes=["cneg2"])
        OP("dve", lambda: nc.vector.tensor_scalar(out=cneg[:], in0=cneg[:], scalar1=-8.0, scalar2=None, op0=ALU.mult),
           reads=["cneg", "cneg2"], writes=["cneg"])

        CH = 1024
        def cast2d(dst, src, rows, cols, key):
            for r0 in range(0, rows, 128):
                for c0 in range(0, cols, CH):
                    c1 = min(cols, c0 + CH)
                    pdma(dst[r0:r0 + 128, c0:c1], src[r0:r0 + 128, c0:c1], [key])
        W0 = w_in[0]
        cast2d(Wfm[:, 0:2048], W0[:, 0:2048], D, 2048, "Wfm")
        cast2d(Wfm[:, 2048:4096], W0[:, 4168:6216], D, 2048, "Wfm")
        for gg in range(2):
            for gsel in range(2):
                for r0 in range(0, D, 256):
                    srcv = W0[r0:r0 + 256, 2048 + gg * 512 + gsel * 256: 2048 + gg * 512 + gsel * 256 + 256].rearrange("r (i d) -> r i d", i=4)
                    dstv = Wtk[r0:r0 + 256, gg * 512:(gg + 1) * 512].rearrange("r (i s d) -> r i s d", i=4, s=2)[:, :, gsel, :]
                    pdma(dstv, srcv, ["Wtk"])
        for gsel in range(2):
            for r0 in range(0, D, 256):
                srcv = W0[r0:r0 + 256, 3584 + gsel * 256: 3584 + gsel * 256 + 256].rearrange("r (i d) -> r i d", i=4)
                dstv = Wtk[r0:r0 + 256, 1024:1536].rearrange("r (i s d) -> r i s d", i=4, s=2)[:, :, gsel, :]
                pdma(dstv, srcv, ["Wtk"])
        cast2d(Wtk[:, 1536:2048], W0[:, 3072:3584], D, 512, "Wtk")
        cast2d(Wtk[:, 2048:2112], W0[:, 4096:4160], D, 64, "Wtk")
        for gsel in range(2):
            for i in range(4):
                pdma(Wtk[:, 2112 + i * 2 + gsel: 2112 + i * 2 + gsel + 1], W0[:, 4160 + gsel * 4 + i: 4160 + gsel * 4 + i + 1], ["Wtk"],
                     allow_slow_non_contiguous=True)
        cast2d(Wrnn, w_rnn_out[0], D, D, "Wrnn")
        cast2d(Watt, w_att_out[0], D, D, "Watt")
        cast2d(Wo, w_o[0], D, D, "Wo")
        cast2d(Wg, w_ffn_gate[0], D, DFF, "Wg")
        cast2d(Wu, w_ffn_up[0], D, DFF, "Wu")
        cast2d(Wd, w_ffn_down[0], DFF, D, "Wd")
        for k_ in ("Wfm", "Wtk", "Wrnn", "Watt", "Wo", "Wg", "Wu", "Wd"):
            Sx.lastw[k_] = (Sx.dsem["prep"], Sx.dcnt["prep"], "dma")
        WKEYS = {"Wfm": Wfm, "Wtk": Wtk, "Wrnn": Wrnn, "Watt": Watt, "Wo": Wo, "Wg": Wg, "Wu": Wu, "Wd": Wd}

        def tile_schedule():
            sch = []
            sch += [("Wtk", "k", 2048, 2120), ("Wtk", "k", 1536, 2048), ("Wtk", "k", 1024, 1536),
                    ("Wtk", "k", 0, 512), ("Wtk", "k", 512, 1024)]
            for g in range(2):
                sch += [("Wfm", "k", g * 512, (g + 1) * 512), ("Wfm", "k", 1024 + g * 512, 1024 + (g + 1) * 512)]
            for g in range(2):
                sch += [("Wrnn", "k", g * 512, (g + 1) * 512), ("Wfm", "k", 2048 + g * 512, 2048 + (g + 1) * 512)]
            for g in range(2):
                sch += [("Watt", "k", g * 512, (g + 1) * 512), ("Wfm", "k", 3072 + g * 512, 3072 + (g + 1) * 512)]
            for g in range(2):
                sch += [("Wo", "k", g * 512, (g + 1) * 512)]
            for g in range(6):
                c0, c1 = g * 512, min(DFF, (g + 1) * 512)
                sch += [("Wg", "k", c0, c1), ("Wu", "k", c0, c1)]
            for g in range(6):
                sch += [("Wd", "d", g * 4, min(FC, (g + 1) * 4))]
            return sch

        SCH = tile_schedule()
        NP = len(SCH)
        TOTAL_TILES = NB * NT
        wstate = {"issued": 0, "used": 0}

        def issue_piece(gidx):
            spec = SCH[gidx % NP]
            b = gidx % NWB
            name, kind, a, bb = spec
            src = WKEYS[name]
            if kind == "k":
                n = bb - a
                dst = wbuf[b][:, 0:KC * n].rearrange("p (k n) -> p k n", k=KC)
                srcv = src[:, a:bb].rearrange("(k p) n -> p k n", p=128)
            else:
                n = bb - a
                dst = wbuf[b][:, 0:n * D].rearrange("p (f n) -> p f n", f=n)
                srcv = src[a * 128:bb * 128, :].rearrange("(f p) n -> p f n", p=128)
            rk = [name] + ([("wstage", w_, t_) for w_ in range(2) for t_ in range(2)] if b == 0 else [])
            Sx.dma("sp", dst, srcv, "wb%d" % b, reads=[name], writes=[("wbuf", b)] + rk[1:])

        def get_piece(expect):
            g = wstate["used"]
            assert SCH[g % NP] == expect, (SCH[g % NP], expect)
            while wstate["issued"] < min(g + 3, TOTAL_TILES * NP):
                issue_piece(wstate["issued"])
                wstate["issued"] += 1
            wstate["used"] += 1
            b = g % NWB
            name, kind, a, bb = expect
            n = bb - a
            if kind == "k":
                view = wbuf[b][:, 0:KC * n].rearrange("p (k n) -> p k n", k=KC)
            else:
                view = wbuf[b][:, 0:n * D].rearrange("p (f n) -> p f n", f=n)
            return view, ("wbuf", b)

        def rms_stats(src_views, src_keys, col0):
            pass

        def evac_transposes(tiles_in, in_keys, dst_fn):
            pass

        fill_reg = nc.gpsimd.to_reg(NEG_FILL)
        try:
          ck("prep", [(g2b[:, 0:512], "g2b"), (cneg[:], "cneg"), (rope[:, 0, :], "rope")])
          for b in range(NB):
              for tt in range(NT):
                  tok0 = tt * T
                  first = (tt == 0)
                  Sx.dma("sp", x_sb[:], x[b, tok0:tok0 + T, :].rearrange("(j p) d -> p j d", p=128), "xld", writes=["x_sb"])
                  for j in range(NJ):
                      OP("act", lambda j=j: nc.scalar.activation(out=xn[:, j, :], in_=x_sb[:, j, :], func=AF.Square,
                                                                  accum_out=stat[:, j:j + 1]),
                         reads=["x_sb"], writes=["xn", ("stat", j)])
                  OP("act", lambda: nc.scalar.activation(out=stat[:, 4:8], in_=stat[:, 0:4], func=AF.Sqrt,
                                                         bias=epsT[:, 0:1], scale=1.0 / D),
                     reads=[("stat", j) for j in range(4)] + ["epsT"], writes=["stat_b"])
                  OP("dve", lambda: nc.vector.reciprocal(out=stat[:, 8:12], in_=stat[:, 4:8]), reads=["stat_b"], writes=["stat_c"])
                  for j in range(NJ):
                      OP("dve", lambda j=j: nc.vector.tensor_scalar(out=xn[:, j, :], in0=x_sb[:, j, :], scalar1=stat[:, 8 + j:9 + j],
                                                                     scalar2=None, op0=ALU.mult),
                         reads=["x_sb", "stat_c"], writes=["xn"])
                  ck("p1a", [(xn[:, 0, 0:512], "xn"), (stat[:, 0:12], "stat_c")])
                  for kc in range(KC):
                      tb = 6 + next_bank("tp", 2)
                      tkey = ("pst", tb)
                      pview = psb[tb][:, 0:512]
                      for j in range(NJ):
                          OP("pe", lambda j=j, kc=kc, pview=pview: nc.tensor.transpose(
                              out=pview[:, j * 128:(j + 1) * 128], in_=xn[:, j, kc * 128:(kc + 1) * 128], identity=ident[:]),
                             reads=["xn", "ident"], writes=[tkey])
                      OP("act", lambda kc=kc, pview=pview: nc.scalar.activation(out=hT[:, kc, :], in_=pview, func=AF.Identity,
                                                                                 scale=g1[:, kc:kc + 1]),
                         reads=[tkey, "g1"], writes=["hT"])

                  ck("p1", [(hT[:, 0, :], "hT"), (hT[:, 7, :], "hT"), (stat[:, 0:12], "stat_c")])
                  def tok_mm(wv, wkey, j, ncols, c0=0):
                      bk = next_bank()
                      for kc in range(KC):
                          OP("pe", lambda kc=kc, bk=bk: nc.tensor.matmul(ps[bk][:, 0:ncols], lhsT=hT[:, kc, j * 128:(j + 1) * 128],
                                                                           rhs=wv[:, kc, c0:c0 + ncols], start=(kc == 0), stop=(kc == KC - 1)),
                             reads=["hT", wkey], writes=[("ps", bk)])
                      tk = next_bank("tok", 2)
                      OP("act", lambda bk=bk, tk=tk: nc.scalar.activation(out=tokp[tk][:, 0:ncols], in_=ps[bk][:, 0:ncols], func=AF.Identity),
                         reads=[("ps", bk)], writes=[("sg", tk)])
                      return tokp[tk], ("sg", tk)

                  def rope_ops(src, skey, nh, blk, dst, dkey):
                      s3 = src.rearrange("p (h d) -> p h d", d=64)
                      d3 = dst.rearrange("p (h d) -> p h d", d=64)
                      cc = rope[:, blk, 0:16].unsqueeze(1).broadcast_to([128, nh, 16])
                      sn_a = rope[:, blk, 16:24].unsqueeze(1).broadcast_to([128, nh, 8])
                      sn_b = rope[:, blk, 24:32].unsqueeze(1).broadcast_to([128, nh, 8])
                      OP("pool", lambda: nc.gpsimd.tensor_copy(out=d3[:, :, 16:64], in_=s3[:, :, 16:64]), reads=[skey], writes=[dkey])
                      OP("dve", lambda: nc.vector.tensor_tensor(out=rtmp[:, 0:nh, :], in0=s3[:, :, 0:16], in1=cc, op=ALU.mult),
                         reads=[skey, "rope"], writes=["rtmp"])
                      OP("dve", lambda: nc.vector.tensor_tensor(out=rtmp2[:, 0:nh, 0:8], in0=s3[:, :, 8:16], in1=sn_a, op=ALU.mult),
                         reads=[skey, "rope"], writes=["rtmp2a"])
                      OP("dve", lambda: nc.vector.tensor_tensor(out=rtmp2[:, 0:nh, 8:16], in0=s3[:, :, 0:8], in1=sn_b, op=ALU.mult),
                         reads=[skey, "rope"], writes=["rtmp2b"])
                      OP("dve", lambda: nc.vector.tensor_tensor(out=d3[:, :, 0:16], in0=rtmp[:, 0:nh, :], in1=rtmp2[:, 0:nh, :], op=ALU.add),
                         reads=["rtmp", "rtmp2a", "rtmp2b"], writes=[dkey])

                  def transposes_to(srcb, skey, nslab, dst_fn, dkeys, only_bank=None):
                      for s0 in range(0, nslab, 4):
                          n = min(4, nslab - s0)
                          tb = only_bank if only_bank is not None else 6 + next_bank("tp", 2)
                          tkey = ("pst", tb)
                          pview = psb[tb][:, 0:n * 128]
                          for s in range(n):
                              OP("pe", lambda s=s, s0=s0, pview=pview: nc.tensor.transpose(
                                  out=pview[:, s * 128:(s + 1) * 128], in_=srcb[:, (s0 + s) * 128:(s0 + s + 1) * 128], identity=ident[:]),
                                 reads=[skey, "ident"], writes=[tkey])
                          dst_fn(s0, n, pview, tkey)

                  tokbufs = [(sg[0], ("sg", 0)), (sg[1], ("sg", 1)), (m1[0], ("m1", 0)), (m1[1], ("m1", 1))]
                  qkbufs = [(qkb, "qkb"), (pT[0], ("pT", 0)), (pT[1], ("pT", 1)), (pT[2], ("pT", 2))]
                  PIECES = [("Wtk", "k", 2048, 2120), ("Wtk", "k", 1536, 2048), ("Wtk", "k", 1024, 1536), ("Wtk", "k", 0, 512), ("Wtk", "k", 512, 1024)]
                  pw = {}

                  def u_S1(u):
                      p, j = u // NJ, u % NJ
                      if j == 0:
                          pw[p] = get_piece(PIECES[p])
                      wv, wkey = pw[p]
                      ncols = PIECES[p][3] - PIECES[p][2]
                      bk = (2, 3, 5, 6)[next_bank("mm", 4)]
                      for kc in range(KC):
                          OP("pe", lambda kc=kc: nc.tensor.matmul(ps[bk][:, 0:ncols], lhsT=hT[:, kc, j * 128:(j + 1) * 128],
                                                                    rhs=wv[:, kc, 0:ncols], start=(kc == 0), stop=(kc == KC - 1)),
                             reads=["hT", wkey], writes=bkeys(bk))
                      tb_, tk_ = tokbufs[u % 4]
                      OP("act", lambda: nc.scalar.activation(out=tb_[:, 0:ncols], in_=ps[bk][:, 0:ncols], func=AF.Identity),
                         reads=bkeys(bk), writes=[tk_])

                  def u_S2(u):
                      p, j = u // NJ, u % NJ
                      blk = tt * NJ + j
                      tp_, tkey_ = tokbufs[u % 4]
                      qb, qkey = qkbufs[u % 4]
                      if p == 0:
                          OP("dve", lambda: nc.vector.bn_stats(out=bnst[:], in_=tp_[:, 0:64]), reads=[tkey_], writes=["bnst"])
                          OP("dve", lambda: nc.vector.bn_aggr(out=bnag[:], in_=bnst[:]), reads=["bnst"], writes=["bnag"])
                          OP("act", lambda: nc.scalar.activation(out=stat[:, 12:13], in_=bnag[:, 1:2], func=AF.Sqrt, bias=epsT[:, 0:1], scale=1.0),
                             reads=["bnag", "epsT"], writes=["lnstd"])
                          wsc = (8 ** -0.5) * (64 ** -0.5)
                          OP("act", lambda: nc.scalar.activation(out=absw[:, j, :], in_=tp_[:, 64:72], func=AF.Abs, scale=wsc),
                             reads=[tkey_], writes=[("absw", j)])
                          OP("act", lambda: nc.scalar.activation(out=sgnw[:, j, :], in_=tp_[:, 64:72], func=AF.Sign),
                             reads=[tkey_], writes=[("sgnw", j)])
                          OP("dve", lambda: nc.vector.reciprocal(out=stat[:, 13:14], in_=stat[:, 12:13]), reads=["lnstd"], writes=["lnrstd"])
                          OP("dve", lambda: nc.vector.tensor_scalar(out=lntmp[:], in0=tp_[:, 0:64], scalar1=bnag[:, 0:1], scalar2=stat[:, 13:14],
                                                                    op0=ALU.subtract, op1=ALU.mult),
                             reads=[tkey_, "bnag", "lnrstd"], writes=["lntmp"])
                          OP("dve", lambda: nc.vector.tensor_tensor(out=lntmp[:], in0=lntmp[:], in1=lngb[:, 0, :], op=ALU.mult),
                             reads=["lntmp", "lng"], writes=["lntmp"])
                          OP("dve", lambda: nc.vector.tensor_tensor(out=lntmp[:], in0=lntmp[:], in1=lngb[:, 1, :], op=ALU.add),
                             reads=["lntmp", "lnb"], writes=["lntmp"])
                          rope_ops(lntmp[:], "lntmp", 1, blk, qb[:, 0:64], qkey)
                          OP("pool", lambda: nc.gpsimd.tensor_copy(out=qb[:, 64:128], in_=qb[:, 0:64]), reads=[qkey], writes=[qkey])
                      elif p == 1:
                          rope_ops(tp_[:, 0:256], tkey_, 4, blk, qb[:, 0:256], qkey)
                          OP("pool", lambda: nc.gpsimd.tensor_copy(out=Vp[:, blk, :, 0:64],
                                                                   in_=tp_[:, 256:512].rearrange("p (g d) -> p g d", g=4)),
                             reads=[tkey_], writes=[("Vp", blk)])
                      elif p == 2:
                          t3 = tp_[:, :].rearrange("p (h d) -> p h d", d=64)
                          OP("dve", lambda: nc.vector.tensor_tensor(out=t3, in0=t3, in1=absw[:, j, :].unsqueeze(2).broadcast_to([128, 8, 64]),
                                                                    op=ALU.mult),
                             reads=[tkey_, ("absw", j)], writes=[tkey_])
                          rope_ops(tp_[:, :], tkey_, 8, blk, qb[:, :], qkey)
                      else:
                          rope_ops(tp_[:, :], tkey_, 8, blk, qb[:, :], qkey)

                  def u_S3(u):
                      p, j = u // NJ, u % NJ
                      blk = tt * NJ + j
                      qb, qkey = qkbufs[u % 4]
                      if p == 0:
                          def dst(s0, n, pview, tkey):
                              OP("act", lambda: nc.scalar.copy(out=ikT2[:, blk * 128:(blk + 1) * 128], in_=pview[:, 0:128]),
                                 reads=[tkey], writes=[("ikT2", blk)])
                          transposes_to(qb, qkey, 1, dst, None)
                      elif p == 1:
                          def dst(s0, n, pview, tkey):
                              OP("act", lambda: nc.scalar.copy(out=KT2[:, 0:2, blk * 128:(blk + 1) * 128],
                                                               in_=pview.rearrange("p (s t) -> p s t", s=2)),
                                 reads=[tkey], writes=[("KT2", blk)])
                          transposes_to(qb, qkey, 2, dst, None)
                      elif p == 2:
                          def dst(s0, n, pview, tkey):
                              pv3 = pview.rearrange("p (s t) -> p s t", s=4)
                              OP("act", lambda: nc.scalar.copy(out=iqz[0:64, 0:4, j * 128:(j + 1) * 128], in_=pv3[0:64]),
                                 reads=[tkey, "iqz0"], writes=[("iqT2", j)])
                              OP("act", lambda: nc.scalar.copy(out=iqz[64:128, 4:8, j * 128:(j + 1) * 128], in_=pv3[64:128]),
                                 reads=[tkey, "iqz0"], writes=[("iqT2", j)])
                          transposes_to(qb, qkey, 4, dst, None)
                      else:
                          half = p - 3

                          def dst(s0, n, pview, tkey):
                              pv3 = pview.rearrange("p (s t) -> p s t", s=4)
                              OP("act", lambda: nc.scalar.copy(out=qz[0:64, half * 4:(half + 1) * 4, j * 128:(j + 1) * 128], in_=pv3[0:64]),
                                 reads=[tkey, "qz0"], writes=[("qT2", j)])
                              OP("act", lambda: nc.scalar.copy(out=qz[64:128, 8 + half * 4:8 + (half + 1) * 4, j * 128:(j + 1) * 128], in_=pv3[64:128]),
                                 reads=[tkey, "qz0"], writes=[("qT2", j)])
                          transposes_to(qb, qkey, 4, dst, None)

                  if first:
                      OP("dve", lambda: nc.vector.memset(halo[:], 0.0), writes=["halo"])
                      OP("dve", lambda: nc.vector.memset(hstate[:], 0.0), writes=["hstate"])
                  p2b_w = {}

                  def p2b_X(c):
                      g, cl = c // 4, c % 4
                      if cl == 0:
                          p2b_w["xr"] = get_piece(("Wfm", "k", g * 512, (g + 1) * 512))
                      wxr, kxr = p2b_w["xr"]
                      bk = next_bank("idx", 2)
                      for kc in range(KC):
                          OP("pe", lambda kc=kc: nc.tensor.matmul(ps[bk][:], lhsT=wxr[:, kc, cl * 128:(cl + 1) * 128], rhs=hT[:, kc, :],
                                                                    start=(kc == 0), stop=(kc == KC - 1)),
                             reads=["hT", kxr], writes=[("ps", bk)])
                      OP("act", lambda: nc.scalar.activation(out=xrh[:, 3:515], in_=ps[bk][:], func=AF.Identity),
                         reads=[("ps", bk)], writes=["xrh"])
                      OP("pool", lambda: nc.gpsimd.tensor_copy(out=xrh[:, 0:3], in_=halo[:, c, :]), reads=["halo"], writes=["xrh"])

                  def p2b_Yc(c):
                      OP("dve", lambda: nc.vector.tensor_scalar(out=xc[:], in0=xrh[:, 0:512], scalar1=cw[:, 0, c:c + 1], scalar2=cb[:, c:c + 1],
                                                                op0=ALU.mult, op1=ALU.add),
                         reads=["xrh", "cw", "cb"], writes=["xc"])
                      for k in range(1, 4):
                          OP("dve", lambda k=k: nc.vector.scalar_tensor_tensor(out=xc[:], in0=xrh[:, k:k + 512], scalar=cw[:, k, c:c + 1],
                                                                               in1=xc[:], op0=ALU.mult, op1=ALU.add),
                             reads=["xrh", "cw", "xc"], writes=["xc"])
                      OP("pool", lambda: nc.gpsimd.tensor_copy(out=halo[:, c, :], in_=xrh[:, 512:515]), reads=["xrh"], writes=["halo"])

                  def p2b_Ya(c):
                      g, cl = c // 4, c % 4
                      if cl == 0:
                          p2b_w["gr"] = get_piece(("Wfm", "k", 1024 + g * 512, 1024 + (g + 1) * 512))
                      wgr, kgr = p2b_w["gr"]
                      bkg = next_bank("idx", 2)
                      for kc in range(KC):
                          OP("pe", lambda kc=kc: nc.tensor.matmul(ps[bkg][:], lhsT=wgr[:, kc, cl * 128:(cl + 1) * 128], rhs=hT[:, kc, :],
                                                                    start=(kc == 0), stop=(kc == KC - 1)),
                             reads=["hT", kgr], writes=[("ps", bkg)])
                      OP("act", lambda: nc.scalar.copy(out=xcb[:], in_=xc[:]), reads=["xc"], writes=["xcb"])
                      OP("act", lambda: nc.scalar.activation(out=gl[:], in_=ps[bkg][:], func=AF.Gelu_apprx_tanh),
                         reads=[("ps", bkg)], writes=[("m1", 1)])
                      bka = next_bank("idx", 2)
                      OP("pe", lambda: nc.tensor.matmul(ps[bka][:], lhsT=Wa_bd[:, c, :], rhs=xcb[:], start=True, stop=True),
                         reads=["Wa_bd", "xcb"], writes=[("ps", bka)])
                      OP("act", lambda: nc.scalar.activation(out=rr, in_=ps[bka][:], func=AF.Sigmoid, bias=ba[:, c:c + 1], scale=1.0),
                         reads=[("ps", bka), "ba"], writes=["rrA"])
                      bkx = next_bank("idx", 2)
                      OP("pe", lambda: nc.tensor.matmul(ps[bkx][:], lhsT=Wx_bd[:, c, :], rhs=xcb[:], start=True, stop=True),
                         reads=["Wx_bd", "xcb"], writes=[("ps", bkx)])
                      OP("act", lambda: nc.scalar.activation(out=ii, in_=ps[bkx][:], func=AF.Sigmoid, bias=bx[:, c:c + 1], scale=1.0),
                         reads=[("ps", bkx), "bx"], writes=["iiA"])
                      OP("act", lambda: nc.scalar.activation(out=aa[:], in_=rr, func=AF.Exp, scale=cneg[:, c:c + 1]),
                         reads=["rrA", "cneg"], writes=[("sg", 0)])
                      OP("act", lambda: nc.scalar.activation(out=ss_[:], in_=rr, func=AF.Exp, scale=cneg2[:, c:c + 1]),
                         reads=["rrA", "cneg2"], writes=[("sg", 1)])
                      OP("act", lambda: nc.scalar.activation(out=ss_[:], in_=ss_[:], func=AF.Sqrt, bias=oneT[:, 0:1], scale=-1.0),
                         reads=[("sg", 1), "oneT"], writes=[("sg", 1)])
                      OP("pool", lambda: nc.gpsimd.tensor_tensor(out=ii, in0=ii, in1=xc[:], op=ALU.mult), reads=["iiA", "xc"], writes=["iiA"])
                      OP("pool", lambda: nc.gpsimd.tensor_tensor(out=ii, in0=ii, in1=ss_[:], op=ALU.mult), reads=["iiA", ("sg", 1)], writes=["iiA"])

                  def p2b_Z(c):
                      OP("dve", lambda: nc.vector.tensor_tensor_scan(out=hh[:], data0=aa[:], data1=ii, initial=hstate[:, c:c + 1],
                                                                     op0=ALU.mult, op1=ALU.add),
                         reads=[("sg", 0), "iiA", "hstate"], writes=[("m1", 0)])
                      OP("pool", lambda: nc.gpsimd.tensor_copy(out=hstate[:, c:c + 1], in_=hh[:, 511:512]), reads=[("m1", 0)], writes=["hstate"])
                      OP("pool", lambda: nc.gpsimd.tensor_tensor(out=yrT[:, c, :], in0=hh[:], in1=gl[:], op=ALU.mult),
                         reads=[("m1", 0), ("m1", 1)], writes=["actT"])

                  def p2b_slot(s_):
                      if 0 <= s_ - 3 < KC:
                          p2b_Z(s_ - 3)
                      if 0 <= s_ - 2 < KC:
                          p2b_Ya(s_ - 2)
                      if 0 <= s_ - 1 < KC:
                          p2b_Yc(s_ - 1)
                      if 0 <= s_ < KC:
                          p2b_X(s_)

                  fillers = [(lambda s_=s_: p2b_slot(s_)) for s_ in range(KC + 3)]
                  mw = {}

                  def merge_unit(pas, fc, bankfn):
                      wname, srcT, gofs = (("Wrnn", yrT, 2048), ("Watt", yattT, 3072))[pas]
                      g, cl = fc // 4, fc % 4
                      if cl == 0:
                          mw["y"] = get_piece((wname, "k", g * 512, (g + 1) * 512))
                          mw["g"] = get_piece(("Wfm", "k", gofs + g * 512, gofs + (g + 1) * 512))
                      wy, ky = mw["y"]
                      wgt, kgt = mw["g"]
                      bky = bankfn()
                      for kc in range(KC):
                          OP("pe", lambda kc=kc: nc.tensor.matmul(ps[bky][:], lhsT=wy[:, kc, cl * 128:(cl + 1) * 128], rhs=srcT[:, kc, :],
                                                                    start=(kc == 0), stop=(kc == KC - 1)),
                             reads=["actT", ky], writes=[("ps", bky)])
                      bkg = bankfn()
                      for kc in range(KC):
                          OP("pe", lambda kc=kc: nc.tensor.matmul(ps[bkg][:], lhsT=wgt[:, kc, cl * 128:(cl + 1) * 128], rhs=hT[:, kc, :],
                                                                    start=(kc == 0), stop=(kc == KC - 1)),
                             reads=["hT", kgt], writes=[("ps", bkg)])
                      si = next_bank("sg", 2)
                      OP("act", lambda: nc.scalar.activation(out=sg[si][:], in_=ps[bkg][:], func=AF.Sigmoid),
                         reads=[("ps", bkg)], writes=[("sg", si)])
                      if pas == 0:
                          OP("dve", lambda: nc.vector.tensor_tensor(out=mergedT[:, fc, :], in0=ps[bky][:], in1=sg[si][:], op=ALU.mult),
                             reads=[("ps", bky), ("sg", si)], writes=["xn"])
                      else:
                          OP("dve", lambda: nc.vector.tensor_tensor(out=m1[si][:], in0=ps[bky][:], in1=sg[si][:], op=ALU.mult),
                             reads=[("ps", bky), ("sg", si)], writes=[("m1", si)])
                          OP("dve", lambda: nc.vector.tensor_tensor(out=mergedT[:, fc, :], in0=mergedT[:, fc, :], in1=m1[si][:], op=ALU.add),
                             reads=[("m1", si), "xn"], writes=["xn"])

                  fillB = [(lambda fc=fc: merge_unit(0, fc, lambda: next_bank("idx", 2))) for fc in range(KC)]
                  def stage_A_units(j):
                      jb = tt * NJ + j
                      n = 128 * (jb + 1)
                      sc, skey = scoreb[j % 2], ("x_sb" if j % 2 == 0 else "score2")
                      units = []

                      def mk_dsg():
                          for hp in range(8):
                              OP("dve", lambda hp=hp: nc.vector.tensor_scalar(out=dsg[:, hp, :], in0=identf[:], scalar1=sgnw[:, j, hp:hp + 1], scalar2=None,
                                                                              op0=ALU.mult),
                                 reads=["identf", ("sgnw", j)], writes=["dsg"])
                      units.append(mk_dsg)
                      nch = (n + 511) // 512
                      for ci in range(nch):
                          c0 = ci * 512
                          cn = min(512, n - c0)
                          ikkeys = [("ikT2", bb_) for bb_ in range(c0 // 128, (c0 + cn) // 128)]
                          st_ = {}

                          def L(hp, c0=c0, cn=cn, ikkeys=ikkeys, st_=st_):
                              if hp == 0:
                                  st_["acc"] = 4
                              half, slot = hp % 2, hp // 2
                              bk = next_bank("idx", 2)
                              OP("pe", lambda: nc.tensor.matmul(
                                  ps[bk][:, 0:cn], lhsT=iqz[:, half * 4 + slot, j * 128:(j + 1) * 128],
                                  rhs=ikT2[:, c0:c0 + cn], start=True, stop=True),
                                 reads=[("iqT2", j)] + ikkeys, writes=[("ps", bk)])
                              ri = next_bank("rh", 2)
                              OP("act", lambda: nc.scalar.activation(out=rh[ri][:, 0:cn], in_=ps[bk][:, 0:cn], func=AF.Relu),
                                 reads=[("ps", bk)], writes=[("rh", ri)])
                              st_[hp] = ri

                          def A(hp, c0=c0, cn=cn, st_=st_):
                              ri = st_[hp]
                              acc = st_["acc"]
                              OP("pe", lambda: nc.tensor.matmul(ps[acc][:, 0:cn], lhsT=dsg[:, hp, :], rhs=rh[ri][:, 0:cn],
                                                                start=(hp == 0), stop=(hp == 7)),
                                 reads=["dsg", ("rh", ri)], writes=bkeys(acc))
                              if hp == 7:
                                  OP("dve", lambda: nc.vector.tensor_copy(out=sc[:, c0:c0 + cn], in_=ps[acc][:, 0:cn]),
                                     reads=bkeys(acc), writes=[skey])
                          units.append(lambda L=L: L(0))
                          for hp in range(8):
                              def u(hp=hp, L=L, A=A):
                                  if hp + 1 < 8:
                                      L(hp + 1)
                                  A(hp)
                              units.append(u)

                      def diag():
                          OP("pool", lambda: nc.gpsimd.affine_select(out=sc[:, jb * 128:(jb + 1) * 128], in_=sc[:, jb * 128:(jb + 1) * 128],
                                                                     pattern=[[-1, 128]], compare_op=ALU.is_ge, fill=fill_reg, base=0,
                                                                     channel_multiplier=1),
                             reads=[skey], writes=[skey])
                      units.append(diag)
                      return units

                  def stage_B_steps(j):
                      jb = tt * NJ + j
                      n = 128 * (jb + 1)
                      sc, skey = scoreb[j % 2], ("x_sb" if j % 2 == 0 else "score2")
                      nmj, nkey = nmb[j % 2], ("xn" if j % 2 == 0 else "nm2")
                      steps = []
                      if n <= TOPK:
                          steps.append(lambda: OP("dve", lambda: nc.vector.memset(bis[:, 3:4], NEG_THR), writes=["thr"]))
                      else:
                          nlo = 128 * jb

                          def init():
                              OP("dve", lambda: nc.vector.tensor_reduce(out=bis[:, 0:1], in_=sc[:, 0:nlo], axis=AX.X, op=ALU.min),
                                 reads=[skey], writes=["b_lo"])
                              OP("dve", lambda: nc.vector.tensor_reduce(out=bis[:, 1:2], in_=sc[:, 0:n], axis=AX.X, op=ALU.max),
                                 reads=[skey], writes=["b_hi"])
                              OP("dve", lambda: nc.vector.tensor_tensor(out=bis[:, 2:3], in0=bis[:, 1:2], in1=bis[:, 0:1], op=ALU.subtract),
                                 reads=["b_lo", "b_hi"], writes=["b_w"])
                              OP("dve", lambda: nc.vector.tensor_scalar(out=wk[:, 0:NITER + 1], in0=pow2[:, 0:NITER + 1], scalar1=bis[:, 2:3], scalar2=None,
                                                                        op0=ALU.mult),
                                 reads=["b_w", "pow2"], writes=["wk"])
                              OP("dve", lambda: nc.vector.tensor_tensor(out=bis[:, 4:5], in0=bis[:, 0:1], in1=wk[:, 0:1], op=ALU.add),
                                 reads=["b_lo", "wk"], writes=[("b_t", 0)])
                          steps.append(init)
                          for k in range(NITER):
                              def it(k=k):
                                  tc_, tn_ = 4 + (k % 2), 4 + ((k + 1) % 2)
                                  OP("dve", lambda: nc.vector.tensor_scalar(out=nmj[:, 0:n], in0=sc[:, 0:n], scalar1=bis[:, tc_:tc_ + 1], scalar2=None,
                                                                            op0=ALU.is_ge, op1=ALU.add, accum_out=bis[:, 6:7]),
                                     reads=[skey, ("b_t", k % 2)], writes=[nkey, "b_cnt"])
                                  OP("dve", lambda: nc.vector.scalar_tensor_tensor(out=bis[:, 7:8], in0=bis[:, 6:7], scalar=float(TOPK), in1=wk[:, k:k + 1],
                                                                                   op0=ALU.is_ge, op1=ALU.mult),
                                     reads=["b_cnt", "wk"], writes=["b_gw"])
                                  OP("dve", lambda: nc.vector.tensor_scalar(out=bis[:, tn_:tn_ + 1], in0=bis[:, 7:8], scalar1=bis[:, tc_:tc_ + 1],
                                                                            scalar2=wk[:, k + 1:k + 2], op0=ALU.add, op1=ALU.subtract),
                                     reads=["b_gw", ("b_t", k % 2), "wk"], writes=[("b_t", (k + 1) % 2)])
                              steps.append(it)

                          def fin():
                              tf = 4 + (NITER % 2)
                              OP("dve", lambda: nc.vector.tensor_tensor(out=bis[:, 3:4], in0=bis[:, tf:tf + 1], in1=wk[:, NITER:NITER + 1], op=ALU.subtract),
                                 reads=[("b_t", NITER % 2), "wk"], writes=["thr"])
                          steps.append(fin)

                      def mask():
                          OP("dve", lambda: nc.vector.tensor_scalar(out=nmj[:, 0:n], in0=sc[:, 0:n], scalar1=bis[:, 3:4], scalar2=MASK_BIG,
                                                                    op0=ALU.is_lt, op1=ALU.mult),
                             reads=[skey, "thr"], writes=[nkey])
                      steps.append(mask)
                      return steps

                  def stage_C(j, bsteps, fill=(), fill2=(), aunits=(), pre=None):
                      jb = tt * NJ + j
                      nmj, nkey = nmb[j % 2], ("xn" if j % 2 == 0 else "nm2")
                      its = [(g, i) for g in range(4) for i in range(jb + 1)]
                      LA = 1
                      state = {}
                      nb_per_g = (len(bsteps) + 3) // 4
                      bpos = [0]

                      def emit_qk(idx):
                          g, i = its[idx]
                          gg, gsel = g // 2, g % 2
                          pa = 2 + next_bank("pa", 2)
                          OP("pe", lambda: nc.tensor.matmul(
                              ps[pa][:], lhsT=KT2[:, gg, i * 128:(i + 1) * 128],
                              rhs=qz[:, gsel * 8 + gg * 4:gsel * 8 + gg * 4 + 4, j * 128:(j + 1) * 128], start=True, stop=False),
                             reads=[("KT2", i), ("qT2", j)], writes=[("ps", pa)])
                          OP("pe", lambda: nc.tensor.matmul(
                              ps[pa][:], lhsT=nmj[:, i * 128:(i + 1) * 128], rhs=ident4[:], start=False, stop=True),
                             reads=[nkey, "ident4"], writes=[("ps", pa)])
                          pi = next_bank("pT", 3)
                          OP("act", lambda: nc.scalar.activation(out=pT[pi][:], in_=ps[pa][:], func=AF.Exp, scale=0.125),
                             reads=[("ps", pa)], writes=[("pT", pi)])
                          state[idx] = pi

                      def emit_pv(idx):
                          g, i = its[idx]
                          if i == 0:
                              state[("po", g)] = 5 + next_bank("po", 2)
                          po = state[("po", g)]
                          pov = ps[po][:, 0:260].rearrange("p (h e) -> p h e", h=4)
                          pi = state[idx]
                          for h in range(4):
                              OP("pe", lambda h=h: nc.tensor.matmul(
                                  pov[:, h, :], lhsT=pT[pi][:, h * 128:(h + 1) * 128], rhs=Vp[:, i, g, :],
                                  start=(i == 0 and h == 0), stop=(i == jb), skip_group_check=True),
                                 reads=[("pT", pi), ("Vp", i), "Vp1"], writes=bkeys(po))
                          if i == jb:
                              for _ in range(nb_per_g):
                                  if bpos[0] < len(bsteps):
                                      bsteps[bpos[0]]()
                                      bpos[0] += 1
                              OP("act", lambda: nc.scalar.copy(out=yatt[:, g * 256:(g + 1) * 256].rearrange("p (h d) -> p h d", h=4), in_=pov[:, :, 0:64]),
                                 reads=bkeys(po), writes=["yatt"])
                              OP("act", lambda: nc.scalar.copy(out=asum[:, g * 4:(g + 1) * 4].unsqueeze(2), in_=pov[:, :, 64:65]),
                                 reads=bkeys(po), writes=["asum"])
                              if j < NJ - 1:
                                  if fill:
                                      fill.pop(0)()
                              else:
                                  while fill:
                                      fill.pop(0)()
                                  for _ in range(2):
                                      if fill2:
                                          fill2.pop(0)()

                      for idx in range(min(LA, len(its))):
                          emit_qk(idx)
                      arate = -(-len(aunits) // max(1, len(its) - 4)) if aunits else 0
                      for idx in range(len(its)):
                          if idx + LA < len(its):
                              emit_qk(idx + LA)
                          if pre is not None and idx == min(7, jb):
                              pre()
                          emit_pv(idx)
                          for _ in range(arate):
                              if aunits:
                                  aunits.pop(0)()
                      while aunits:
                          aunits.pop(0)()
                      while bpos[0] < len(bsteps):
                          bsteps[bpos[0]]()
                          bpos[0] += 1

                      def finish():
                          OP("dve", lambda: nc.vector.reciprocal(out=arec[:], in_=asum[:]), reads=["asum"], writes=["arec"])
                          OP("dve", lambda: nc.vector.tensor_tensor(
                              out=yatt[:].rearrange("p (h d) -> p h d", h=16), in0=yatt[:].rearrange("p (h d) -> p h d", h=16),
                              in1=arec[:].unsqueeze(2).broadcast_to([128, 16, 64]), op=ALU.mult),
                             reads=["yatt", "arec"], writes=["yatt"])

                          def ya_dst(s0, n_, pview, tkey):
                              OP("act", lambda: nc.scalar.copy(out=yattT[:, s0:s0 + n_, j * 128:(j + 1) * 128],
                                                               in_=pview.rearrange("p (s t) -> p s t", s=n_)),
                                 reads=[tkey], writes=["actT"])
                          transposes_to(yatt, "yatt", 8, ya_dst, None, only_bank=7)
                      return finish

                  NU = 5 * NJ
                  b0steps = None
                  a1 = None
                  for step in range(NU + 2):
                      if step < NU:
                          u_S1(step)
                      if 0 <= step - 1 < NU:
                          u_S2(step - 1)
                      if 0 <= step - 2 < NU:
                          u_S3(step - 2)
                      if step == 2 * NJ + 2:
                          for f_ in stage_A_units(0):
                              f_()
                          b0steps = stage_B_steps(0)
                      elif b0steps is not None:
                          if b0steps:
                              b0steps.pop(0)()
                          if step == 2 * NJ + 3:
                              a1 = stage_A_units(1)
                          if a1:
                              for _ in range(4):
                                  if a1:
                                      a1.pop(0)()
                  k_ = 0
                  while b0steps:
                      b0steps.pop(0)()
                      k_ += 1
                      for _ in range(4):
                          if a1:
                              a1.pop(0)()
                      if k_ % 2 == 1 and fillers:
                          fillers.pop(0)()
                  while a1:
                      a1.pop(0)()
                  while fillers:
                      fillers.pop(0)()
                  fin_prev = None
                  for j in range(NJ):
                      bst = stage_B_steps(j + 1) if j + 1 < NJ else []
                      aun = stage_A_units(j + 2) if j + 2 < NJ else []
                      fin_prev = stage_C(j, bst, fillers, fillB, aun, pre=fin_prev)
                  fin_prev()
                  while fillers:
                      fillers.pop(0)()
                  while fillB:
                      fillB.pop(0)()
                  ck("p2b", [(yrT[:, 0, :], "actT"), (yrT[:, 7, :], "actT")])

                  ck("p3", [(yattT[:, 0, :], "actT"), (yattT[:, 7, :], "actT")])
                  Sx.dma("sp", x_sb[:], x[b, tok0:tok0 + T, :].rearrange("(j p) d -> p j d", p=128), "xld", writes=["x_sb"])
                  for fc in range(KC):
                      merge_unit(1, fc, next_bank)
                  wo_p = [get_piece(("Wo", "k", g * 512, (g + 1) * 512)) for g in range(2)]

                  def post_norm_residual(banks, j, gbt, gkey):
                      for hf in range(2):
                          OP("act", lambda hf=hf: nc.scalar.activation(out=m1[hf][:], in_=ps[banks[hf]][:], func=AF.Square,
                                                                       accum_out=stat[:, hf:hf + 1]),
                             reads=bkeys(banks[hf]), writes=[("m1", hf), ("stat", hf)])
                      OP("dve", lambda: nc.vector.tensor_tensor(out=stat[:, 2:3], in0=stat[:, 0:1], in1=stat[:, 1:2], op=ALU.add),
                         reads=[("stat", 0), ("stat", 1)], writes=[("stat", 2)])
                      OP("act", lambda: nc.scalar.activation(out=stat[:, 4:5], in_=stat[:, 2:3], func=AF.Sqrt, bias=epsT[:, 0:1], scale=1.0 / D),
                         reads=[("stat", 2), "epsT"], writes=["stat_b"])
                      OP("dve", lambda: nc.vector.reciprocal(out=stat[:, 8:9], in_=stat[:, 4:5]), reads=["stat_b"], writes=["stat_c"])
                      for hf in range(2):
                          OP("dve", lambda hf=hf: nc.vector.scalar_tensor_tensor(out=m1[hf][:], in0=ps[banks[hf]][:], scalar=stat[:, 8:9],
                                                                                  in1=gbt[:, hf * 512:(hf + 1) * 512], op0=ALU.mult, op1=ALU.mult),
                             reads=bkeys(banks[hf]) + ["stat_c", gkey], writes=[("m1", hf)])
                          OP("dve", lambda hf=hf: nc.vector.tensor_tensor(out=x_sb[:, j, hf * 512:(hf + 1) * 512], in0=x_sb[:, j, hf * 512:(hf + 1) * 512],
                                                                          in1=m1[hf][:], op=ALU.add),
                             reads=[("m1", hf), "x_sb"], writes=["x_sb"])

                  for j in range(NJ):
                      banks = [next_bank(), next_bank()]
                      for hf in range(2):
                          wv_, wk_ = wo_p[hf]
                          for kc in range(KC):
                              OP("pe", lambda kc=kc, hf=hf, wv_=wv_: nc.tensor.matmul(ps[banks[hf]][:], lhsT=mergedT[:, kc, j * 128:(j + 1) * 128],
                                                                                       rhs=wv_[:, kc, :], start=(kc == 0), stop=(kc == KC - 1)),
                                 reads=["xn", wk_], writes=[("ps", banks[hf])])
                      post_norm_residual(banks, j, g2b, "g2b")

                  ck("p4", [(x_sb[:].rearrange("p j d -> p (j d)"), "x_sb")])
                  for j in range(NJ):
                      OP("act", lambda j=j: nc.scalar.activation(out=xn[:, j, :], in_=x_sb[:, j, :], func=AF.Square, accum_out=stat[:, j:j + 1]),
                         reads=["x_sb"], writes=["xn", ("stat", j)])
                  OP("act", lambda: nc.scalar.activation(out=stat[:, 4:8], in_=stat[:, 0:4], func=AF.Sqrt, bias=epsT[:, 0:1], scale=1.0 / D),
                     reads=[("stat", j) for j in range(4)] + ["epsT"], writes=["stat_b"])
                  OP("dve", lambda: nc.vector.reciprocal(out=stat[:, 8:12], in_=stat[:, 4:8]), reads=["stat_b"], writes=["stat_c"])
                  for j in range(NJ):
                      OP("dve", lambda j=j: nc.vector.tensor_scalar(out=xn[:, j, :], in0=x_sb[:, j, :], scalar1=stat[:, 8 + j:9 + j], scalar2=None, op0=ALU.mult),
                         reads=["x_sb", "stat_c"], writes=["xn"])
                  for kc in range(KC):
                      tb = 6 + next_bank("tp", 2)
                      tkey = ("pst", tb)
                      pview = psb[tb][:, 0:512]
                      for j in range(NJ):
                          OP("pe", lambda j=j, kc=kc, pview=pview: nc.tensor.transpose(
                              out=pview[:, j * 128:(j + 1) * 128], in_=xn[:, j, kc * 128:(kc + 1) * 128], identity=ident[:]),
                             reads=["xn", "ident"], writes=[tkey])
                      OP("act", lambda kc=kc, pview=pview: nc.scalar.activation(out=hT[:, kc, :], in_=pview, func=AF.Identity, scale=g3[:, kc:kc + 1]),
                         reads=[tkey, "g3"], writes=["hT"])
                  for g in range(6):
                      c0, c1 = g * 512, min(DFF, (g + 1) * 512)
                      wgv, kgv = get_piece(("Wg", "k", c0, c1))
                      wuv, kuv = get_piece(("Wu", "k", c0, c1))
                      for cl in range((c1 - c0) // 128):
                          fc = g * 4 + cl
                          bg = next_bank()
                          for kc in range(KC):
                              OP("pe", lambda kc=kc, bg=bg: nc.tensor.matmul(ps[bg][:], lhsT=wgv[:, kc, cl * 128:(cl + 1) * 128], rhs=hT[:, kc, :],
                                                                              start=(kc == 0), stop=(kc == KC - 1)),
                                 reads=["hT", kgv], writes=[("ps", bg)])
                          bu = next_bank()
                          for kc in range(KC):
                              OP("pe", lambda kc=kc, bu=bu: nc.tensor.matmul(ps[bu][:], lhsT=wuv[:, kc, cl * 128:(cl + 1) * 128], rhs=hT[:, kc, :],
                                                                              start=(kc == 0), stop=(kc == KC - 1)),
                                 reads=["hT", kuv], writes=[("ps", bu)])
                          si = next_bank("sg", 2)
                          OP("act", lambda bg=bg, si=si: nc.scalar.activation(out=sg[si][:], in_=ps[bg][:], func=AF.Silu),
                             reads=[("ps", bg)], writes=[("sg", si)])
                          OP("dve", lambda bu=bu, si=si, fc=fc: nc.vector.tensor_tensor(out=actT[:, fc, :], in0=ps[bu][:], in1=sg[si][:], op=ALU.mult),
                             reads=[("ps", bu), ("sg", si)], writes=["actT", "rrA", "iiA", "ptmpA"])
                  for g in range(6):
                      f0, f1 = g * 4, min(FC, (g + 1) * 4)
                      wdv, kdv = get_piece(("Wd", "d", f0, f1))
                      for fl in range(f1 - f0):
                          fc = f0 + fl
                          for j in range(NJ):
                              for hf in range(2):
                                  bk = j * 2 + hf
                                  OP("pe", lambda fl=fl, fc=fc, j=j, hf=hf, bk=bk: nc.tensor.matmul(
                                      ps[bk][:], lhsT=actT[:, fc, j * 128:(j + 1) * 128], rhs=wdv[:, fl, hf * 512:(hf + 1) * 512],
                                      start=(fc == 0), stop=(fc == FC - 1)),
                                     reads=["actT", "rrA", "iiA", "ptmpA", kdv], writes=bkeys(bk))
                  for j in range(NJ):
                      post_norm_residual([j * 2, j * 2 + 1], j, g4b, "g4b")
                  Sx.dma("sp", out[b, tok0:tok0 + T, :].rearrange("(j p) d -> p j d", p=128), x_sb[:], "ost", reads=["x_sb"])

        except _Stop:
            pass
        if "ost" in Sx.dsem:
            nc.sync.wait_ge(Sx.dsem["ost"], Sx.dcnt["ost"])
        if "dbg" in Sx.dsem:
            nc.sync.wait_ge(Sx.dsem["dbg"], Sx.dcnt["dbg"])
        stats = dict(ninstr=Sx.ninstr, nwaits=Sx.nwaits, nsem=len(Sx.dsem) + 4)
    return nc, stats


def _consts(S):
    half = 8
    inv_freq = (500000.0 ** (-np.arange(half, dtype=np.float32) / half)).astype(np.float32)
    ang = (np.arange(S, dtype=np.float32)[:, None] * inv_freq[None, :]).astype(np.float32)
    cos = np.cos(ang).astype(np.float32)
    sin = np.sin(ang).astype(np.float32)
    rope = np.concatenate([cos, cos, -sin, sin], axis=1).astype(np.float32)
    ident = np.eye(128, dtype=np.float32)
    pow2 = np.tile((2.0 ** -(np.arange(64, dtype=np.float32) + 1.0))[None, :], (128, 1)).astype(np.float32)
    return rope, ident, pow2


_CACHE = {}


def kernel(**inputs):
    x = np.ascontiguousarray(inputs["x"], dtype=np.float32)
    B, S, _ = x.shape
    ncores = 8
    NB = B // ncores
    key = (NB, S)
    if key not in _CACHE:
        _CACHE[key] = build_program(NB, S)
    nc, stats = _CACHE[key]
    rope, ident, pow2 = _consts(S)
    in_maps = []
    for c in range(ncores):
        m = {k: np.ascontiguousarray(v, dtype=np.float32) for k, v in inputs.items() if k != "x"}
        m["x"] = x[c * NB:(c + 1) * NB]
        m["rope_tab"] = rope
        m["ident_in"] = ident
        m["pow2_in"] = pow2
        in_maps.append(m)
    res = run_bass_kernel_spmd(nc, in_maps, core_ids=list(range(ncores)))
    return np.concatenate([np.asarray(r["out"]) for r in res.results], axis=0).astype(np.float32)
```

```python
import contextlib
import math
import numpy as np
import concourse.bass as bass
import concourse.mybir as mybir
from concourse.bass_utils import run_bass_kernel_spmd

F32 = mybir.dt.float32
BF16 = mybir.dt.bfloat16
AF = mybir.ActivationFunctionType
ALU = mybir.AluOpType
AX = mybir.AxisListType

D = 1024
KC = 8
T = 512
NJ = 4
DFF = 2816
FC = 22
DIN = 6216
EPS = 1e-6
NEG_FILL = -1.0e30
NEG_THR = -1.0e29
MASK_BIG = -30000.0


class Sched:
    ENG = ("pe", "act", "dve", "pool", "sp")

    def __init__(self, nc, stack):
        self.nc = nc
        self.stack = stack
        self.e = {"pe": nc.tensor, "act": nc.scalar, "dve": nc.vector, "pool": nc.gpsimd, "sp": nc.sync}
        self.psem = {}
        self.cnt = {}
        for n in ("pe", "act", "dve", "pool"):
            self.psem[n] = stack.enter_context(nc.semaphore("prog_" + n))
            self.cnt[n] = 0
        self.known = {n: {} for n in self.ENG}
        self.lastw = {}
        self.readers = {}
        self.dsem = {}
        self.dcnt = {}
        self.slot_keys = {}
        self.nwaits = 0
        self.ninstr = 0

    def _deps(self, eng, reads, writes):
        deps = {}

        def add(p):
            s, v = p[0], p[1]
            if id(s) not in deps or deps[id(s)][1] < v:
                deps[id(s)] = (s, v)

        for k in reads:
            p = self.lastw.get(k)
            if p is not None and not (p[2] == eng and eng == "pe"):
                add(p)
        for k in writes:
            p = self.lastw.get(k)
            if p is not None and not (p[2] == eng and eng == "pe"):
                add(p)
            for p in self.readers.get(k, {}).values():
                if not (p[2] == eng and eng == "pe"):
                    add(p)
        return deps

    def _emit_waits(self, eng, deps):
        kn = self.known[eng]
        for s, v in deps.values():
            if kn.get(id(s), 0) >= v:
                continue
            self.e[eng].wait_ge(s, v)
            kn[id(s)] = v
            self.nwaits += 1

    def _record(self, reads, writes, token):
        for k in reads:
            self.readers.setdefault(k, {})[id(token[0])] = token
        for k in writes:
            self.lastw[k] = token
            self.readers[k] = {}

    def op(self, eng, fn, reads=(), writes=()):
        deps = self._deps(eng, reads, writes)
        self._emit_waits(eng, deps)
        ins = fn()
        self.cnt[eng] += 1
        ins.then_inc(self.psem[eng], 1)
        self._record(reads, writes, (self.psem[eng], self.cnt[eng], eng))
        self.ninstr += 1
        return ins

    def dma(self, queue, out, in_, slot, reads=(), writes=(), **kw):
        if slot not in self.dsem:
            self.dsem[slot] = self.stack.enter_context(self.nc.semaphore("d_" + str(slot)))
            self.dcnt[slot] = 0
        deps = self._deps(queue, reads, writes)
        self._emit_waits(queue, deps)
        ins = self.e[queue].dma_start(out=out, in_=in_, **kw)
        self.dcnt[slot] += 16
        ins.then_inc(self.dsem[slot], 16)
        self._record(reads, writes, (self.dsem[slot], self.dcnt[slot], "dma"))
        self.slot_keys.setdefault(slot, set()).update(writes)
        self.ninstr += 1
        return ins

    def seal(self, slot):
        for k in self.slot_keys.get(slot, ()):
            self.lastw[k] = (self.dsem[slot], self.dcnt[slot], "dma")

    def wait_keys(self, eng, keys):
        deps = self._deps(eng, keys, ())
        self._emit_waits(eng, deps)


class _Stop(Exception):
    pass


def build_program(NB, S, NITER=16, TOPK=256, debug=None):
    NT = S // T
    NBLK = S // 128
    nc = bass.Bass("TRN2", target_bir_lowering=False, dynamic_dma_scratch_size=4096)
    dt = lambda name, shape, dtype=F32, kind="ExternalInput": nc.dram_tensor(name, shape, dtype, kind=kind).ap()
    x = dt("x", [NB, S, D])
    out = dt("out", [NB, S, D], kind="ExternalOutput")
    norm_mix_pre = dt("norm_mix_pre", [1, D])
    w_in = dt("w_in", [1, D, DIN])
    conv_w = dt("conv_w", [1, 4, D])
    conv_b = dt("conv_b", [1, D])
    rg_w_a = dt("rg_w_a", [1, 16, 64, 64])
    rg_b_a = dt("rg_b_a", [1, D])
    rg_w_x = dt("rg_w_x", [1, 16, 64, 64])
    rg_b_x = dt("rg_b_x", [1, D])
    rg_lambda = dt("rg_lambda", [1, D])
    idx_k_ln_g = dt("idx_k_ln_g", [1, 64])
    idx_k_ln_b = dt("idx_k_ln_b", [1, 64])
    w_rnn_out = dt("w_rnn_out", [1, D, D])
    w_att_out = dt("w_att_out", [1, D, D])
    w_o = dt("w_o", [1, D, D])
    norm_mix_post = dt("norm_mix_post", [1, D])
    norm_ffn_pre = dt("norm_ffn_pre", [1, D])
    w_ffn_gate = dt("w_ffn_gate", [1, D, DFF])
    w_ffn_up = dt("w_ffn_up", [1, D, DFF])
    w_ffn_down = dt("w_ffn_down", [1, DFF, D])
    norm_ffn_post = dt("norm_ffn_post", [1, D])
    rope_tab = dt("rope_tab", [S, 32])
    ident_in = dt("ident_in", [128, 128])
    pow2_in = dt("pow2_in", [128, 64])

    Wfm = dt("Wfm", [D, 4096], BF16, kind="Internal")
    Wtk = dt("Wtk", [D, 2120], BF16, kind="Internal")
    Wrnn = dt("Wrnn", [D, D], BF16, kind="Internal")
    Watt = dt("Watt", [D, D], BF16, kind="Internal")
    Wo = dt("Wo", [D, D], BF16, kind="Internal")
    Wg = dt("Wg", [D, DFF], BF16, kind="Internal")
    Wu = dt("Wu", [D, DFF], BF16, kind="Internal")
    Wd = dt("Wd", [DFF, D], BF16, kind="Internal")

    dbg = dt("dbg", [128, 4096], kind="ExternalOutput") if debug else None
    st = contextlib.ExitStack()
    with st:
        Sx = Sched(nc, st)

        def ck(name, dumps=()):
            if debug != name:
                return
            col = 0
            for di, (ap, key) in enumerate(dumps):
                n = ap.shape[-1]
                if ap.dtype == BF16:
                    stg = st.enter_context(nc.sbuf_tensor("dbgst%d" % di, [128, n], F32))
                    OP("act", lambda: nc.scalar.copy(out=stg[0:ap.shape[0], :], in_=ap), reads=key if isinstance(key, list) else [key], writes=[("dbgst", di)])
                    Sx.dma("sp", dbg[0:ap.shape[0], col:col + n], stg[0:ap.shape[0], :], "dbg", reads=[("dbgst", di)])
                else:
                    Sx.dma("sp", dbg[0:ap.shape[0], col:col + n], ap, "dbg", reads=key if isinstance(key, list) else [key])
                col += n
            raise _Stop()

        sb = lambda n, s, d=F32: st.enter_context(nc.sbuf_tensor(n, s, d))
        OP = Sx.op

        x_sb = sb("x_sb", [128, NJ, D])
        score = x_sb[:].rearrange("p j d -> p (j d)")
        xn = sb("xn", [128, NJ, D], BF16)
        nm = xn[:].rearrange("p j d -> p (j d)")
        score2 = sb("score2", [128, 4096])
        nm2 = sb("nm2", [128, 4096], BF16)
        scoreb = [score, score2]
        nmb = [nm, nm2]
        mergedT = xn[:].rearrange("p j d -> p (j d)").rearrange("p (k t) -> p k t", k=KC)
        hT = sb("hT", [128, KC, T], BF16)
        actT = sb("actT", [128, FC, T], BF16)
        yrT = actT[:, 0:8, :]
        yattT = actT[:, 8:16, :]
        NWB = 4
        wbuf = [sb("wbuf%d" % i, [128, 4096], BF16) for i in range(NWB)]
        wstage = wbuf[0][:].bitcast(F32).rearrange("p (w c j) -> p w c j", w=2, c=KC)
        KT2 = sb("KT2", [128, 2, S], BF16)
        ikT2 = sb("ikT2", [128, S], BF16)
        Vp = sb("Vp", [128, NBLK, 4, 65], BF16)
        qz = sb("qz", [128, 16, T], BF16)
        iqz = sb("iqz", [128, 8, T], BF16)
        absw = sb("absw", [128, NJ, 8])
        sgnw = sb("sgnw", [128, NJ, 8])
        ident = sb("ident", [128, 128], BF16)
        ident4 = sb("ident4", [128, 4, 128], BF16)
        identf = sb("identf", [128, 128])
        g1 = sb("g1", [128, KC])
        g3 = sb("g3", [128, KC])
        g2b = sb("g2b", [128, D])
        g4b = sb("g4b", [128, D])
        lngb = sb("lngb", [128, 2, 64])
        cw = sb("cw", [128, 4, KC])
        cb = sb("cb", [128, KC])
        ba = sb("ba", [128, KC])
        bx = sb("bx", [128, KC])
        lam = sb("lam", [128, KC])
        cneg = sb("cneg", [128, KC])
        cneg2 = sb("cneg2", [128, KC])
        Wa_bd = sb("Wa_bd", [128, KC, 128], BF16)
        Wx_bd = sb("Wx_bd", [128, KC, 128], BF16)
        rope = sb("rope", [128, NBLK, 32])
        pow2 = sb("pow2", [128, 64])
        epsT = sb("epsT", [128, 1])
        oneT = sb("oneT", [128, 1])
        halo = sb("halo", [128, KC, 3])
        hstate = sb("hstate", [128, KC])
        xrh = sb("xrh", [128, 515])
        xc = sb("xc", [128, 512])
        xcb = sb("xcb", [128, 512], BF16)
        qkb = sb("qkb", [128, 512], BF16)
        rtmp = sb("rtmp", [128, 8, 16])
        rtmp2 = sb("rtmp2", [128, 8, 16])
        rh = [sb("rh%d" % i, [128, 512], BF16) for i in range(2)]
        dsg = sb("dsg", [128, 8, 128], BF16)
        pT = [sb("pT%d" % i, [128, 512], BF16) for i in range(3)]
        yatt = sb("yatt", [128, D], BF16)
        sg = [sb("sg%d" % i, [128, 512]) for i in range(2)]
        m1 = [sb("m1_%d" % i, [128, 512]) for i in range(2)]
        tokp = sg
        actflat = actT[:].rearrange("p f t -> p (f t)")
        rr = actflat[:, 16 * T:18 * T].bitcast(F32)
        ii = actflat[:, 18 * T:20 * T].bitcast(F32)
        ptmp = actflat[:, 20 * T:22 * T].bitcast(F32)
        aa, ss_, hh, gl = sg[0], sg[1], m1[0], m1[1]
        stat = sb("stat", [128, 16])
        bnst = sb("bnst", [128, 6])
        bnag = sb("bnag", [128, 2])
        lntmp = sb("lntmp", [128, 64])
        bis = sb("bis", [128, 8])
        wk = sb("wk", [128, 64])
        rec = sb("rec", [128, 4])
        asum = sb("asum", [128, 16])
        arec = sb("arec", [128, 16])

        ps = [st.enter_context(nc.psum_tensor("ps%d" % i, [128, 512], F32)) for i in range(8)]
        psb = [p[:].bitcast(BF16) for p in ps]

        rr_ctr = {"mm": 0, "tp": 0, "tok": 0, "rh": 0, "pT": 0, "sg": 0, "po": 0, "idx": 0, "pa": 0, "acc": 0}

        def bkeys(bk):
            return [("ps", bk)] if bk < 6 else [("pst", bk)]

        def next_bank(pool="mm", n=6):
            i = rr_ctr[pool] % n
            rr_ctr[pool] += 1
            return i
        next_bank_ = next_bank

        cq = 0
        def pdma(out_ap, in_ap, wkeys, **kw):
            nonlocal cq
            cq += 1
            Sx.dma("pool", out_ap, in_ap, "prep", writes=[], **kw)

        def sdma(out_ap, in_ap, wkeys, **kw):
            nonlocal cq
            cq += 1
            Sx.dma("sp", out_ap, in_ap, "cst%d" % cq, writes=wkeys, **kw)

        sdma(identf[:], ident_in, ["identf"])
        sdma(pow2[:], pow2_in, ["pow2"])
        sdma(rope[:], rope_tab.rearrange("(b p) c -> p b c", p=128), ["rope"])
        sdma(g2b[:], norm_mix_post[0].partition_broadcast(128), ["g2b"])
        sdma(g4b[:], norm_ffn_post[0].partition_broadcast(128), ["g4b"])
        sdma(lngb[:, 0, :], idx_k_ln_g[0].partition_broadcast(128), ["lng"])
        sdma(lngb[:, 1, :], idx_k_ln_b[0].partition_broadcast(128), ["lnb"])
        for tile_, src, k_ in ((g1, norm_mix_pre, "g1"), (g3, norm_ffn_pre, "g3"), (cb, conv_b, "cb"), (ba, rg_b_a, "ba"),
                               (bx, rg_b_x, "bx"), (lam, rg_lambda, "lam")):
            sdma(tile_[:], src[0].rearrange("(c p) -> p c", p=128), [k_], allow_slow_non_contiguous=True)
        sdma(cw[:], conv_w[0].rearrange("k (c p) -> p k c", p=128), ["cw"], allow_slow_non_contiguous=True)
        for wi, (wsrc, wdst, nm_) in enumerate(((rg_w_a, Wa_bd, "Wa_bd"), (rg_w_x, Wx_bd, "Wx_bd"))):
            OP("pool", lambda wdst=wdst: nc.gpsimd.memset(wdst[:], 0.0), writes=[nm_])
            v = wsrc[0].rearrange("(c two) i j -> two i c j", two=2)
            for two in range(2):
                Sx.dma("sp", wstage[two * 64:(two + 1) * 64, wi, :, 0:64], v[two], "wst%d%d" % (wi, two), writes=[("wstage", wi, two)])
                OP("act", lambda two=two, wi=wi, wdst=wdst: nc.scalar.activation(
                    out=wdst[two * 64:(two + 1) * 64, :, two * 64:(two + 1) * 64],
                    in_=wstage[two * 64:(two + 1) * 64, wi, :, 0:64], func=AF.Identity),
                   reads=[("wstage", wi, two)], writes=[nm_])
        OP("pool", lambda: nc.gpsimd.memset(Vp[:, :, :, 64:65], 1.0), writes=["Vp1"])
        OP("pool", lambda: nc.gpsimd.memset(qz[:], 0.0), writes=["qz0"])
        OP("pool", lambda: nc.gpsimd.memset(iqz[:], 0.0), writes=["iqz0"])
        OP("dve", lambda: nc.vector.memset(epsT[:], EPS), writes=["epsT"])
        OP("dve", lambda: nc.vector.memset(oneT[:], 1.0), writes=["oneT"])
        OP("dve", lambda: nc.vector.tensor_copy(out=ident[:], in_=identf[:]), reads=["identf"], writes=["ident"])
        for r in range(4):
            OP("dve", lambda r=r: nc.vector.tensor_copy(out=ident4[:, r, :], in_=identf[:]), reads=["identf"], writes=["ident4"])
        OP("act", lambda: nc.scalar.activation(out=cneg[:], in_=lam[:], func=AF.Exp, scale=-1.0), reads=["lam"], writes=["cneg"])
        OP("act", lambda: nc.scalar.activation(out=cneg[:], in_=cneg[:], func=AF.Ln, bias=oneT[:, 0:1], scale=1.0),
           reads=["cneg", "oneT"], writes=["cneg"])
        OP("dve", lambda: nc.vector.tensor_scalar(out=cneg2[:], in0=cneg[:], scalar1=-16.0, scalar2=None, op0=ALU.mult),
           reads=["cneg"], writes=["cneg2"])
        OP("dve", lambda: nc.vector.tensor_scalar(out=cneg[:], in0=cneg[:], scalar1=-8.0, scalar2=None, op0=ALU.mult),
           reads=["cneg", "cneg2"], writes=["cneg"])

        CH = 1024
        def cast2d(dst, src, rows, cols, key):
            for r0 in range(0, rows, 128):
                for c0 in range(0, cols, CH):
                    c1 = min(cols, c0 + CH)
                    pdma(dst[r0:r0 + 128, c0:c1], src[r0:r0 + 128, c0:c1], [key])
        W0 = w_in[0]
        cast2d(Wfm[:, 0:2048], W0[:, 0:2048], D, 2048, "Wfm")
        cast2d(Wfm[:, 2048:4096], W0[:, 4168:6216], D, 2048, "Wfm")
        for gg in range(2):
            for gsel in range(2):
                for r0 in range(0, D, 256):
                    srcv = W0[r0:r0 + 256, 2048 + gg * 512 + gsel * 256: 2048 + gg * 512 + gsel * 256 + 256].rearrange("r (i d) -> r i d", i=4)
                    dstv = Wtk[r0:r0 + 256, gg * 512:(gg + 1) * 512].rearrange("r (i s d) -> r i s d", i=4, s=2)[:, :, gsel, :]
                    pdma(dstv, srcv, ["Wtk"])
        for gsel in range(2):
            for r0 in range(0, D, 256):
                srcv = W0[r0:r0 + 256, 3584 + gsel * 256: 3584 + gsel * 256 + 256].rearrange("r (i d) -> r i d", i=4)
                dstv = Wtk[r0:r0 + 256, 1024:1536].rearrange("r (i s d) -> r i s d", i=4, s=2)[:, :, gsel, :]
                pdma(dstv, srcv, ["Wtk"])
        cast2d(Wtk[:, 1536:2048], W0[:, 3072:3584], D, 512, "Wtk")
        cast2d(Wtk[:, 2048:2112], W0[:, 4096:4160], D, 64, "Wtk")
        for gsel in range(2):
            for i in range(4):
                pdma(Wtk[:, 2112 + i * 2 + gsel: 2112 + i * 2 + gsel + 1], W0[:, 4160 + gsel * 4 + i: 4160 + gsel * 4 + i + 1], ["Wtk"],
                     allow_slow_non_contiguous=True)
        cast2d(Wrnn, w_rnn_out[0], D, D, "Wrnn")
        cast2d(Watt, w_att_out[0], D, D, "Watt")
        cast2d(Wo, w_o[0], D, D, "Wo")
        cast2d(Wg, w_ffn_gate[0], D, DFF, "Wg")
        cast2d(Wu, w_ffn_up[0], D, DFF, "Wu")
        cast2d(Wd, w_ffn_down[0], DFF, D, "Wd")
        for k_ in ("Wfm", "Wtk", "Wrnn", "Watt", "Wo", "Wg", "Wu", "Wd"):
            Sx.lastw[k_] = (Sx.dsem["prep"], Sx.dcnt["prep"], "dma")
        WKEYS = {"Wfm": Wfm, "Wtk": Wtk, "Wrnn": Wrnn, "Watt": Watt, "Wo": Wo, "Wg": Wg, "Wu": Wu, "Wd": Wd}

        def tile_schedule():
            sch = []
            sch += [("Wtk", "k", 2048, 2120), ("Wtk", "k", 1536, 2048), ("Wtk", "k", 1024, 1536),
                    ("Wtk", "k", 0, 512), ("Wtk", "k", 512, 1024)]
            for g in range(2):
                sch += [("Wfm", "k", g * 512, (g + 1) * 512), ("Wfm", "k", 1024 + g * 512, 1024 + (g + 1) * 512)]
            for g in range(2):
                sch += [("Wrnn", "k", g * 512, (g + 1) * 512), ("Wfm", "k", 2048 + g * 512, 2048 + (g + 1) * 512)]
            for g in range(2):
                sch += [("Watt", "k", g * 512, (g + 1) * 512), ("Wfm", "k", 3072 + g * 512, 3072 + (g + 1) * 512)]
            for g in range(2):
                sch += [("Wo", "k", g * 512, (g + 1) * 512)]
            for g in range(6):
                c0, c1 = g * 512, min(DFF, (g + 1) * 512)
                sch += [("Wg", "k", c0, c1), ("Wu", "k", c0, c1)]
            for g in range(6):
                sch += [("Wd", "d", g * 4, min(FC, (g + 1) * 4))]
            return sch

        SCH = tile_schedule()
        NP = len(SCH)
        TOTAL_TILES = NB * NT
        wstate = {"issued": 0, "used": 0}

        def issue_piece(gidx):
            spec = SCH[gidx % NP]
            b = gidx % NWB
            name, kind, a, bb = spec
            src = WKEYS[name]
            if kind == "k":
                n = bb - a
                dst = wbuf[b][:, 0:KC * n].rearrange("p (k n) -> p k n", k=KC)
                srcv = src[:, a:bb].rearrange("(k p) n -> p k n", p=128)
            else:
                n = bb - a
                dst = wbuf[b][:, 0:n * D].rearrange("p (f n) -> p f n", f=n)
                srcv = src[a * 128:bb * 128, :].rearrange("(f p) n -> p f n", p=128)
            rk = [name] + ([("wstage", w_, t_) for w_ in range(2) for t_ in range(2)] if b == 0 else [])
            Sx.dma("sp", dst, srcv, "wb%d" % b, reads=[name], writes=[("wbuf", b)] + rk[1:])

        def get_piece(expect):
            g = wstate["used"]
            assert SCH[g % NP] == expect, (SCH[g % NP], expect)
            while wstate["issued"] < min(g + 3, TOTAL_TILES * NP):
                issue_piece(wstate["issued"])
                wstate["issued"] += 1
            wstate["used"] += 1
            b = g % NWB
            name, kind, a, bb = expect
            n = bb - a
            if kind == "k":
                view = wbuf[b][:, 0:KC * n].rearrange("p (k n) -> p k n", k=KC)
            else:
                view = wbuf[b][:, 0:n * D].rearrange("p (f n) -> p f n", f=n)
            return view, ("wbuf", b)

        def rms_stats(src_views, src_keys, col0):
            pass

        def evac_transposes(tiles_in, in_keys, dst_fn):
            pass

        fill_reg = nc.gpsimd.to_reg(NEG_FILL)
        try:
          ck("prep", [(g2b[:, 0:512], "g2b"), (cneg[:], "cneg"), (rope[:, 0, :], "rope")])
          for b in range(NB):
              for tt in range(NT):
                  tok0 = tt * T
                  first = (tt == 0)
                  Sx.dma("sp", x_sb[:], x[b, tok0:tok0 + T, :].rearrange("(j p) d -> p j d", p=128), "xld", writes=["x_sb"])
                  for j in range(NJ):
                      OP("act", lambda j=j: nc.scalar.activation(out=xn[:, j, :], in_=x_sb[:, j, :], func=AF.Square,
                                                                  accum_out=stat[:, j:j + 1]),
                         reads=["x_sb"], writes=["xn", ("stat", j)])
                  OP("act", lambda: nc.scalar.activation(out=stat[:, 4:8], in_=stat[:, 0:4], func=AF.Sqrt,
                                                         bias=epsT[:, 0:1], scale=1.0 / D),
                     reads=[("stat", j) for j in range(4)] + ["epsT"], writes=["stat_b"])
                  OP("dve", lambda: nc.vector.reciprocal(out=stat[:, 8:12], in_=stat[:, 4:8]), reads=["stat_b"], writes=["stat_c"])
                  for j in range(NJ):
                      OP("dve", lambda j=j: nc.vector.tensor_scalar(out=xn[:, j, :], in0=x_sb[:, j, :], scalar1=stat[:, 8 + j:9 + j],
                                                                     scalar2=None, op0=ALU.mult),
                         reads=["x_sb", "stat_c"], writes=["xn"])
                  ck("p1a", [(xn[:, 0, 0:512], "xn"), (stat[:, 0:12], "stat_c")])
                  for kc in range(KC):
                      tb = 6 + next_bank("tp", 2)
                      tkey = ("pst", tb)
                      pview = psb[tb][:, 0:512]
                      for j in range(NJ):
                          OP("pe", lambda j=j, kc=kc, pview=pview: nc.tensor.transpose(
                              out=pview[:, j * 128:(j + 1) * 128], in_=xn[:, j, kc * 128:(kc + 1) * 128], identity=ident[:]),
                             reads=["xn", "ident"], writes=[tkey])
                      OP("act", lambda kc=kc, pview=pview: nc.scalar.activation(out=hT[:, kc, :], in_=pview, func=AF.Identity,
                                                                                 scale=g1[:, kc:kc + 1]),
                         reads=[tkey, "g1"], writes=["hT"])

                  ck("p1", [(hT[:, 0, :], "hT"), (hT[:, 7, :], "hT"), (stat[:, 0:12], "stat_c")])
                  def tok_mm(wv, wkey, j, ncols, c0=0):
                      bk = next_bank()
                      for kc in range(KC):
                          OP("pe", lambda kc=kc, bk=bk: nc.tensor.matmul(ps[bk][:, 0:ncols], lhsT=hT[:, kc, j * 128:(j + 1) * 128],
                                                                           rhs=wv[:, kc, c0:c0 + ncols], start=(kc == 0), stop=(kc == KC - 1)),
                             reads=["hT", wkey], writes=[("ps", bk)])
                      tk = next_bank("tok", 2)
                      OP("act", lambda bk=bk, tk=tk: nc.scalar.activation(out=tokp[tk][:, 0:ncols], in_=ps[bk][:, 0:ncols], func=AF.Identity),
                         reads=[("ps", bk)], writes=[("sg", tk)])
                      return tokp[tk], ("sg", tk)

                  def rope_ops(src, skey, nh, blk, dst, dkey):
                      s3 = src.rearrange("p (h d) -> p h d", d=64)
                      d3 = dst.rearrange("p (h d) -> p h d", d=64)
                      cc = rope[:, blk, 0:16].unsqueeze(1).broadcast_to([128, nh, 16])
                      sn_a = rope[:, blk, 16:24].unsqueeze(1).broadcast_to([128, nh, 8])
                      sn_b = rope[:, blk, 24:32].unsqueeze(1).broadcast_to([128, nh, 8])
                      OP("pool", lambda: nc.gpsimd.tensor_copy(out=d3[:, :, 16:64], in_=s3[:, :, 16:64]), reads=[skey], writes=[dkey])
                      OP("dve", lambda: nc.vector.tensor_tensor(out=rtmp[:, 0:nh, :], in0=s3[:, :, 0:16], in1=cc, op=ALU.mult),
                         reads=[skey, "rope"], writes=["rtmp"])
                      OP("dve", lambda: nc.vector.tensor_tensor(out=rtmp2[:, 0:nh, 0:8], in0=s3[:, :, 8:16], in1=sn_a, op=ALU.mult),
                         reads=[skey, "rope"], writes=["rtmp2a"])
                      OP("dve", lambda: nc.vector.tensor_tensor(out=rtmp2[:, 0:nh, 8:16], in0=s3[:, :, 0:8], in1=sn_b, op=ALU.mult),
                         reads=[skey, "rope"], writes=["rtmp2b"])
                      OP("dve", lambda: nc.vector.tensor_tensor(out=d3[:, :, 0:16], in0=rtmp[:, 0:nh, :], in1=rtmp2[:, 0:nh, :], op=ALU.add),
                         reads=["rtmp", "rtmp2a", "rtmp2b"], writes=[dkey])

                  def transposes_to(srcb, skey, nslab, dst_fn, dkeys, only_bank=None):
                      for s0 in range(0, nslab, 4):
                          n = min(4, nslab - s0)
                          tb = only_bank if only_bank is not None else 6 + next_bank("tp", 2)
                          tkey = ("pst", tb)
                          pview = psb[tb][:, 0:n * 128]
                          for s in range(n):
                              OP("pe", lambda s=s, s0=s0, pview=pview: nc.tensor.transpose(
                                  out=pview[:, s * 128:(s + 1) * 128], in_=srcb[:, (s0 + s) * 128:(s0 + s + 1) * 128], identity=ident[:]),
                                 reads=[skey, "ident"], writes=[tkey])
                          dst_fn(s0, n, pview, tkey)

                  tokbufs = [(sg[0], ("sg", 0)), (sg[1], ("sg", 1)), (m1[0], ("m1", 0)), (m1[1], ("m1", 1))]
                  qkbufs = [(qkb, "qkb"), (pT[0], ("pT", 0)), (pT[1], ("pT", 1)), (pT[2], ("pT", 2))]
                  PIECES = [("Wtk", "k", 2048, 2120), ("Wtk", "k", 1536, 2048), ("Wtk", "k", 1024, 1536), ("Wtk", "k", 0, 512), ("Wtk", "k", 512, 1024)]
                  pw = {}

                  def u_S1(u):
                      p, j = u // NJ, u % NJ
                      if j == 0:
                          pw[p] = get_piece(PIECES[p])
                      wv, wkey = pw[p]
                      ncols = PIECES[p][3] - PIECES[p][2]
                      bk = (2, 3, 5, 6)[next_bank("mm", 4)]
                      for kc in range(KC):
                          OP("pe", lambda kc=kc: nc.tensor.matmul(ps[bk][:, 0:ncols], lhsT=hT[:, kc, j * 128:(j + 1) * 128],
                                                                    rhs=wv[:, kc, 0:ncols], start=(kc == 0), stop=(kc == KC - 1)),
                             reads=["hT", wkey], writes=bkeys(bk))
                      tb_, tk_ = tokbufs[u % 4]
                      OP("act", lambda: nc.scalar.activation(out=tb_[:, 0:ncols], in_=ps[bk][:, 0:ncols], func=AF.Identity),
                         reads=bkeys(bk), writes=[tk_])

                  def u_S2(u):
                      p, j = u // NJ, u % NJ
                      blk = tt * NJ + j
                      tp_, tkey_ = tokbufs[u % 4]
                      qb, qkey = qkbufs[u % 4]
                      if p == 0:
                          OP("dve", lambda: nc.vector.bn_stats(out=bnst[:], in_=tp_[:, 0:64]), reads=[tkey_], writes=["bnst"])
                          OP("dve", lambda: nc.vector.bn_aggr(out=bnag[:], in_=bnst[:]), reads=["bnst"], writes=["bnag"])
                          OP("act", lambda: nc.scalar.activation(out=stat[:, 12:13], in_=bnag[:, 1:2], func=AF.Sqrt, bias=epsT[:, 0:1], scale=1.0),
                             reads=["bnag", "epsT"], writes=["lnstd"])
                          wsc = (8 ** -0.5) * (64 ** -0.5)
                          OP("act", lambda: nc.scalar.activation(out=absw[:, j, :], in_=tp_[:, 64:72], func=AF.Abs, scale=wsc),
                             reads=[tkey_], writes=[("absw", j)])
                          OP("act", lambda: nc.scalar.activation(out=sgnw[:, j, :], in_=tp_[:, 64:72], func=AF.Sign),
                             reads=[tkey_], writes=[("sgnw", j)])
                          OP("dve", lambda: nc.vector.reciprocal(out=stat[:, 13:14], in_=stat[:, 12:13]), reads=["lnstd"], writes=["lnrstd"])
                          OP("dve", lambda: nc.vector.tensor_scalar(out=lntmp[:], in0=tp_[:, 0:64], scalar1=bnag[:, 0:1], scalar2=stat[:, 13:14],
                                                                    op0=ALU.subtract, op1=ALU.mult),
                             reads=[tkey_, "bnag", "lnrstd"], writes=["lntmp"])
                          OP("dve", lambda: nc.vector.tensor_tensor(out=lntmp[:], in0=lntmp[:], in1=lngb[:, 0, :], op=ALU.mult),
                             reads=["lntmp", "lng"], writes=["lntmp"])
                          OP("dve", lambda: nc.vector.tensor_tensor(out=lntmp[:], in0=lntmp[:], in1=lngb[:, 1, :], op=ALU.add),
                             reads=["lntmp", "lnb"], writes=["lntmp"])
                          rope_ops(lntmp[:], "lntmp", 1, blk, qb[:, 0:64], qkey)
                          OP("pool", lambda: nc.gpsimd.tensor_copy(out=qb[:, 64:128], in_=qb[:, 0:64]), reads=[qkey], writes=[qkey])
                      elif p == 1:
                          rope_ops(tp_[:, 0:256], tkey_, 4, blk, qb[:, 0:256], qkey)
                          OP("pool", lambda: nc.gpsimd.tensor_copy(out=Vp[:, blk, :, 0:64],
                                                                   in_=tp_[:, 256:512].rearrange("p (g d) -> p g d", g=4)),
                             reads=[tkey_], writes=[("Vp", blk)])
                      elif p == 2:
                          t3 = tp_[:, :].rearrange("p (h d) -> p h d", d=64)
                          OP("dve", lambda: nc.vector.tensor_tensor(out=t3, in0=t3, in1=absw[:, j, :].unsqueeze(2).broadcast_to([128, 8, 64]),
                                                                    op=ALU.mult),
                             reads=[tkey_, ("absw", j)], writes=[tkey_])
                          rope_ops(tp_[:, :], tkey_, 8, blk, qb[:, :], qkey)
                      else:
                          rope_ops(tp_[:, :], tkey_, 8, blk, qb[:, :], qkey)

                  def u_S3(u):
                      p, j = u // NJ, u % NJ
                      blk = tt * NJ + j
                      qb, qkey = qkbufs[u % 4]
                      if p == 0:
                          def dst(s0, n, pview, tkey):
                              OP("act", lambda: nc.scalar.copy(out=ikT2[:, blk * 128:(blk + 1) * 128], in_=pview[:, 0:128]),
                                 reads=[tkey], writes=[("ikT2", blk)])
                          transposes_to(qb, qkey, 1, dst, None)
                      elif p == 1:
                          def dst(s0, n, pview, tkey):
                              OP("act", lambda: nc.scalar.copy(out=KT2[:, 0:2, blk * 128:(blk + 1) * 128],
                                                               in_=pview.rearrange("p (s t) -> p s t", s=2)),
                                 reads=[tkey], writes=[("KT2", blk)])
                          transposes_to(qb, qkey, 2, dst, None)
                      elif p == 2:
                          def dst(s0, n, pview, tkey):
                              pv3 = pview.rearrange("p (s t) -> p s t", s=4)
                              OP("act", lambda: nc.scalar.copy(out=iqz[0:64, 0:4, j * 128:(j + 1) * 128], in_=pv3[0:64]),
                                 reads=[tkey, "iqz0"], writes=[("iqT2", j)])
                              OP("act", lambda: nc.scalar.copy(out=iqz[64:128, 4:8, j * 128:(j + 1) * 128], in_=pv3[64:128]),
                                 reads=[tkey, "iqz0"], writes=[("iqT2", j)])
                          transposes_to(qb, qkey, 4, dst, None)
                      else:
                          half = p - 3

                          def dst(s0, n, pview, tkey):
                              pv3 = pview.rearrange("p (s t) -> p s t", s=4)
                              OP("act", lambda: nc.scalar.copy(out=qz[0:64, half * 4:(half + 1) * 4, j * 128:(j + 1) * 128], in_=pv3[0:64]),
                                 reads=[tkey, "qz0"], writes=[("qT2", j)])
                              OP("act", lambda: nc.scalar.copy(out=qz[64:128, 8 + half * 4:8 + (half + 1) * 4, j * 128:(j + 1) * 128], in_=pv3[64:128]),
                                 reads=[tkey, "qz0"], writes=[("qT2", j)])
                          transposes_to(qb, qkey, 4, dst, None)

                  if first:
                      OP("dve", lambda: nc.vector.memset(halo[:], 0.0), writes=["halo"])
                      OP("dve", lambda: nc.vector.memset(hstate[:], 0.0), writes=["hstate"])
                  p2b_w = {}

                  def p2b_X(c):
                      g, cl = c // 4, c % 4
                      if cl == 0:
                          p2b_w["xr"] = get_piece(("Wfm", "k", g * 512, (g + 1) * 512))
                      wxr, kxr = p2b_w["xr"]
                      bk = next_bank("idx", 2)
                      for kc in range(KC):
                          OP("pe", lambda kc=kc: nc.tensor.matmul(ps[bk][:], lhsT=wxr[:, kc, cl * 128:(cl + 1) * 128], rhs=hT[:, kc, :],
                                                                    start=(kc == 0), stop=(kc == KC - 1)),
                             reads=["hT", kxr], writes=[("ps", bk)])
                      OP("act", lambda: nc.scalar.activation(out=xrh[:, 3:515], in_=ps[bk][:], func=AF.Identity),
                         reads=[("ps", bk)], writes=["xrh"])
                      OP("pool", lambda: nc.gpsimd.tensor_copy(out=xrh[:, 0:3], in_=halo[:, c, :]), reads=["halo"], writes=["xrh"])

                  def p2b_Yc(c):
                      OP("dve", lambda: nc.vector.tensor_scalar(out=xc[:], in0=xrh[:, 0:512], scalar1=cw[:, 0, c:c + 1], scalar2=cb[:, c:c + 1],
                                                                op0=ALU.mult, op1=ALU.add),
                         reads=["xrh", "cw", "cb"], writes=["xc"])
                      for k in range(1, 4):
                          OP("dve", lambda k=k: nc.vector.scalar_tensor_tensor(out=xc[:], in0=xrh[:, k:k + 512], scalar=cw[:, k, c:c + 1],
                                                                               in1=xc[:], op0=ALU.mult, op1=ALU.add),
                             reads=["xrh", "cw", "xc"], writes=["xc"])
                      OP("pool", lambda: nc.gpsimd.tensor_copy(out=halo[:, c, :], in_=xrh[:, 512:515]), reads=["xrh"], writes=["halo"])

                  def p2b_Ya(c):
                      g, cl = c // 4, c % 4
                      if cl == 0:
                          p2b_w["gr"] = get_piece(("Wfm", "k", 1024 + g * 512, 1024 + (g + 1) * 512))
                      wgr, kgr = p2b_w["gr"]
                      bkg = next_bank("idx", 2)
                      for kc in range(KC):
                          OP("pe", lambda kc=kc: nc.tensor.matmul(ps[bkg][:], lhsT=wgr[:, kc, cl * 128:(cl + 1) * 128], rhs=hT[:, kc, :],
                                                                    start=(kc == 0), stop=(kc == KC - 1)),
                             reads=["hT", kgr], writes=[("ps", bkg)])
                      OP("act", lambda: nc.scalar.copy(out=xcb[:], in_=xc[:]), reads=["xc"], writes=["xcb"])
                      OP("act", lambda: nc.scalar.activation(out=gl[:], in_=ps[bkg][:], func=AF.Gelu_apprx_tanh),
                         reads=[("ps", bkg)], writes=[("m1", 1)])
                      bka = next_bank("idx", 2)
                      OP("pe", lambda: nc.tensor.matmul(ps[bka][:], lhsT=Wa_bd[:, c, :], rhs=xcb[:], start=True, stop=True),
                         reads=["Wa_bd", "xcb"], writes=[("ps", bka)])
                      OP("act", lambda: nc.scalar.activation(out=rr, in_=ps[bka][:], func=AF.Sigmoid, bias=ba[:, c:c + 1], scale=1.0),
                         reads=[("ps", bka), "ba"], writes=["rrA"])
                      bkx = next_bank("idx", 2)
                      OP("pe", lambda: nc.tensor.matmul(ps[bkx][:], lhsT=Wx_bd[:, c, :], rhs=xcb[:], start=True, stop=True),
                         reads=["Wx_bd", "xcb"], writes=[("ps", bkx)])
                      OP("act", lambda: nc.scalar.activation(out=ii, in_=ps[bkx][:], func=AF.Sigmoid, bias=bx[:, c:c + 1], scale=1.0),
                         reads=[("ps", bkx), "bx"], writes=["iiA"])
                      OP("act", lambda: nc.scalar.activation(out=aa[:], in_=rr, func=AF.Exp, scale=cneg[:, c:c + 1]),
                         reads=["rrA", "cneg"], writes=[("sg", 0)])
                      OP("act", lambda: nc.scalar.activation(out=ss_[:], in_=rr, func=AF.Exp, scale=cneg2[:, c:c + 1]),
                         reads=["rrA", "cneg2"], writes=[("sg", 1)])
                      OP("act", lambda: nc.scalar.activation(out=ss_[:], in_=ss_[:], func=AF.Sqrt, bias=oneT[:, 0:1], scale=-1.0),
                         reads=[("sg", 1), "oneT"], writes=[("sg", 1)])
                      OP("pool", lambda: nc.gpsimd.tensor_tensor(out=ii, in0=ii, in1=xc[:], op=ALU.mult), reads=["iiA", "xc"], writes=["iiA"])
                      OP("pool", lambda: nc.gpsimd.tensor_tensor(out=ii, in0=ii, in1=ss_[:], op=ALU.mult), reads=["iiA", ("sg", 1)], writes=["iiA"])

                  def p2b_Z(c):
                      OP("dve", lambda: nc.vector.tensor_tensor_scan(out=hh[:], data0=aa[:], data1=ii, initial=hstate[:, c:c + 1],
                                                                     op0=ALU.mult, op1=ALU.add),
                         reads=[("sg", 0), "iiA", "hstate"], writes=[("m1", 0)])
                      OP("pool", lambda: nc.gpsimd.tensor_copy(out=hstate[:, c:c + 1], in_=hh[:, 511:512]), reads=[("m1", 0)], writes=["hstate"])
                      OP("pool", lambda: nc.gpsimd.tensor_tensor(out=yrT[:, c, :], in0=hh[:], in1=gl[:], op=ALU.mult),
                         reads=[("m1", 0), ("m1", 1)], writes=["actT"])

                  def p2b_slot(s_):
                      if 0 <= s_ - 3 < KC:
                          p2b_Z(s_ - 3)
                      if 0 <= s_ - 2 < KC:
                          p2b_Ya(s_ - 2)
                      if 0 <= s_ - 1 < KC:
                          p2b_Yc(s_ - 1)
                      if 0 <= s_ < KC:
                          p2b_X(s_)

                  fillers = [(lambda s_=s_: p2b_slot(s_)) for s_ in range(KC + 3)]
                  mw = {}

                  def merge_unit(pas, fc, bankfn):
                      wname, srcT, gofs = (("Wrnn", yrT, 2048), ("Watt", yattT, 3072))[pas]
                      g, cl = fc // 4, fc % 4
                      if cl == 0:
                          mw["y"] = get_piece((wname, "k", g * 512, (g + 1) * 512))
                          mw["g"] = get_piece(("Wfm", "k", gofs + g * 512, gofs + (g + 1) * 512))
                      wy, ky = mw["y"]
                      wgt, kgt = mw["g"]
                      bky = bankfn()
                      for kc in range(KC):
                          OP("pe", lambda kc=kc: nc.tensor.matmul(ps[bky][:], lhsT=wy[:, kc, cl * 128:(cl + 1) * 128], rhs=srcT[:, kc, :],
                                                                    start=(kc == 0), stop=(kc == KC - 1)),
                             reads=["actT", ky], writes=[("ps", bky)])
                      bkg = bankfn()
                      for kc in range(KC):
                          OP("pe", lambda kc=kc: nc.tensor.matmul(ps[bkg][:], lhsT=wgt[:, kc, cl * 128:(cl + 1) * 128], rhs=hT[:, kc, :],
                                                                    start=(kc == 0), stop=(kc == KC - 1)),
                             reads=["hT", kgt], writes=[("ps", bkg)])
                      si = next_bank("sg", 2)
                      OP("act", lambda: nc.scalar.activation(out=sg[si][:], in_=ps[bkg][:], func=AF.Sigmoid),
                         reads=[("ps", bkg)], writes=[("sg", si)])
                      if pas == 0:
                          OP("dve", lambda: nc.vector.tensor_tensor(out=mergedT[:, fc, :], in0=ps[bky][:], in1=sg[si][:], op=ALU.mult),
                             reads=[("ps", bky), ("sg", si)], writes=["xn"])
                      else:
                          OP("dve", lambda: nc.vector.tensor_tensor(out=m1[si][:], in0=ps[bky][:], in1=sg[si][:], op=ALU.mult),
                             reads=[("ps", bky), ("sg", si)], writes=[("m1", si)])
                          OP("dve", lambda: nc.vector.tensor_tensor(out=mergedT[:, fc, :], in0=mergedT[:, fc, :], in1=m1[si][:], op=ALU.add),
                             reads=[("m1", si), "xn"], writes=["xn"])

                  fillB = [(lambda fc=fc: merge_unit(0, fc, lambda: next_bank("idx", 2))) for fc in range(KC)]
                  def stage_A_units(j):
                      jb = tt * NJ + j
                      n = 128 * (jb + 1)
                      sc, skey = scoreb[j % 2], ("x_sb" if j % 2 == 0 else "score2")
                      units = []

                      def mk_dsg():
                          for hp in range(8):
                              OP("dve", lambda hp=hp: nc.vector.tensor_scalar(out=dsg[:, hp, :], in0=identf[:], scalar1=sgnw[:, j, hp:hp + 1], scalar2=None,
                                                                              op0=ALU.mult),
                                 reads=["identf", ("sgnw", j)], writes=["dsg"])
                      units.append(mk_dsg)
                      nch = (n + 511) // 512
                      for ci in range(nch):
                          c0 = ci * 512
                          cn = min(512, n - c0)
                          ikkeys = [("ikT2", bb_) for bb_ in range(c0 // 128, (c0 + cn) // 128)]
                          st_ = {}

                          def L(hp, c0=c0, cn=cn, ikkeys=ikkeys, st_=st_):
                              if hp == 0:
                                  st_["acc"] = 4
                              half, slot = hp % 2, hp // 2
                              bk = next_bank("idx", 2)
                              OP("pe", lambda: nc.tensor.matmul(
                                  ps[bk][:, 0:cn], lhsT=iqz[:, half * 4 + slot, j * 128:(j + 1) * 128],
                                  rhs=ikT2[:, c0:c0 + cn], start=True, stop=True),
                                 reads=[("iqT2", j)] + ikkeys, writes=[("ps", bk)])
                              ri = next_bank("rh", 2)
                              OP("act", lambda: nc.scalar.activation(out=rh[ri][:, 0:cn], in_=ps[bk][:, 0:cn], func=AF.Relu),
                                 reads=[("ps", bk)], writes=[("rh", ri)])
                              st_[hp] = ri

                          def A(hp, c0=c0, cn=cn, st_=st_):
                              ri = st_[hp]
                              acc = st_["acc"]
                              OP("pe", lambda: nc.tensor.matmul(ps[acc][:, 0:cn], lhsT=dsg[:, hp, :], rhs=rh[ri][:, 0:cn],
                                                                start=(hp == 0), stop=(hp == 7)),
                                 reads=["dsg", ("rh", ri)], writes=bkeys(acc))
                              if hp == 7:
                                  OP("act", lambda: nc.scalar.copy(out=sc[:, c0:c0 + cn], in_=ps[acc][:, 0:cn]),
                                     reads=bkeys(acc), writes=[skey])
                          units.append(lambda L=L: L(0))
                          for hp in range(8):
                              def u(hp=hp, L=L, A=A):
                                  if hp + 1 < 8:
                                      L(hp + 1)
                                  A(hp)
                              units.append(u)

                      def diag():
                          OP("pool", lambda: nc.gpsimd.affine_select(out=sc[:, jb * 128:(jb + 1) * 128], in_=sc[:, jb * 128:(jb + 1) * 128],
                                                                     pattern=[[-1, 128]], compare_op=ALU.is_ge, fill=fill_reg, base=0,
                                                                     channel_multiplier=1),
                             reads=[skey], writes=[skey])
                      units.append(diag)
                      return units

                  def stage_B_steps(j):
                      jb = tt * NJ + j
                      n = 128 * (jb + 1)
                      sc, skey = scoreb[j % 2], ("x_sb" if j % 2 == 0 else "score2")
                      nmj, nkey = nmb[j % 2], ("xn" if j % 2 == 0 else "nm2")
                      steps = []
                      if n <= TOPK:
                          steps.append(lambda: OP("dve", lambda: nc.vector.memset(bis[:, 3:4], NEG_THR), writes=["thr"]))
                      else:
                          nlo = 128 * jb

                          def init():
                              OP("dve", lambda: nc.vector.tensor_reduce(out=bis[:, 0:1], in_=sc[:, 0:nlo], axis=AX.X, op=ALU.min),
                                 reads=[skey], writes=["b_lo"])
                              OP("dve", lambda: nc.vector.tensor_reduce(out=bis[:, 1:2], in_=sc[:, 0:n], axis=AX.X, op=ALU.max),
                                 reads=[skey], writes=["b_hi"])
                              OP("dve", lambda: nc.vector.tensor_tensor(out=bis[:, 2:3], in0=bis[:, 1:2], in1=bis[:, 0:1], op=ALU.subtract),
                                 reads=["b_lo", "b_hi"], writes=["b_w"])
                              OP("dve", lambda: nc.vector.tensor_scalar(out=wk[:, 0:NITER + 1], in0=pow2[:, 0:NITER + 1], scalar1=bis[:, 2:3], scalar2=None,
                                                                        op0=ALU.mult),
                                 reads=["b_w", "pow2"], writes=["wk"])
                              OP("dve", lambda: nc.vector.tensor_tensor(out=bis[:, 4:5], in0=bis[:, 0:1], in1=wk[:, 0:1], op=ALU.add),
                                 reads=["b_lo", "wk"], writes=[("b_t", 0)])
                          steps.append(init)
                          for k in range(NITER):
                              def it(k=k):
                                  tc_, tn_ = 4 + (k % 2), 4 + ((k + 1) % 2)
                                  OP("dve", lambda: nc.vector.tensor_scalar(out=nmj[:, 0:n], in0=sc[:, 0:n], scalar1=bis[:, tc_:tc_ + 1], scalar2=None,
                                                                            op0=ALU.is_ge, op1=ALU.add, accum_out=bis[:, 6:7]),
                                     reads=[skey, ("b_t", k % 2)], writes=[nkey, "b_cnt"])
                                  OP("dve", lambda: nc.vector.scalar_tensor_tensor(out=bis[:, 7:8], in0=bis[:, 6:7], scalar=float(TOPK), in1=wk[:, k:k + 1],
                                                                                   op0=ALU.is_ge, op1=ALU.mult),
                                     reads=["b_cnt", "wk"], writes=["b_gw"])
                                  OP("dve", lambda: nc.vector.tensor_scalar(out=bis[:, tn_:tn_ + 1], in0=bis[:, 7:8], scalar1=bis[:, tc_:tc_ + 1],
                                                                            scalar2=wk[:, k + 1:k + 2], op0=ALU.add, op1=ALU.subtract),
                                     reads=["b_gw", ("b_t", k % 2), "wk"], writes=[("b_t", (k + 1) % 2)])
                              steps.append(it)

                          def fin():
                              tf = 4 + (NITER % 2)
                              OP("dve", lambda: nc.vector.tensor_tensor(out=bis[:, 3:4], in0=bis[:, tf:tf + 1], in1=wk[:, NITER:NITER + 1], op=ALU.subtract),
                                 reads=[("b_t", NITER % 2), "wk"], writes=["thr"])
                          steps.append(fin)

                      def mask():
                          OP("dve", lambda: nc.vector.tensor_scalar(out=nmj[:, 0:n], in0=sc[:, 0:n], scalar1=bis[:, 3:4], scalar2=MASK_BIG,
                                                                    op0=ALU.is_lt, op1=ALU.mult),
                             reads=[skey, "thr"], writes=[nkey])
                      steps.append(mask)
                      return steps

                  def stage_C(j, bsteps, fill=(), fill2=(), aunits=(), pre=None):
                      jb = tt * NJ + j
                      nmj, nkey = nmb[j % 2], ("xn" if j % 2 == 0 else "nm2")
                      its = [(g, i) for g in range(4) for i in range(jb + 1)]
                      LA = 1
                      state = {}
                      nb_per_g = (len(bsteps) + 3) // 4
                      bpos = [0]

                      def emit_qk(idx):
                          g, i = its[idx]
                          gg, gsel = g // 2, g % 2
                          pa = 2 + next_bank("pa", 2)
                          OP("pe", lambda: nc.tensor.matmul(
                              ps[pa][:], lhsT=KT2[:, gg, i * 128:(i + 1) * 128],
                              rhs=qz[:, gsel * 8 + gg * 4:gsel * 8 + gg * 4 + 4, j * 128:(j + 1) * 128], start=True, stop=False),
                             reads=[("KT2", i), ("qT2", j)], writes=[("ps", pa)])
                          OP("pe", lambda: nc.tensor.matmul(
                              ps[pa][:], lhsT=nmj[:, i * 128:(i + 1) * 128], rhs=ident4[:], start=False, stop=True),
                             reads=[nkey, "ident4"], writes=[("ps", pa)])
                          pi = next_bank("pT", 3)
                          OP("act", lambda: nc.scalar.activation(out=pT[pi][:], in_=ps[pa][:], func=AF.Exp, scale=0.125),
                             reads=[("ps", pa)], writes=[("pT", pi)])
                          state[idx] = pi

                      def emit_pv(idx):
                          g, i = its[idx]
                          if i == 0:
                              state[("po", g)] = 5 + next_bank("po", 2)
                          po = state[("po", g)]
                          pov = ps[po][:, 0:260].rearrange("p (h e) -> p h e", h=4)
                          pi = state[idx]
                          for h in range(4):
                              OP("pe", lambda h=h: nc.tensor.matmul(
                                  pov[:, h, :], lhsT=pT[pi][:, h * 128:(h + 1) * 128], rhs=Vp[:, i, g, :],
                                  start=(i == 0 and h == 0), stop=(i == jb), skip_group_check=True),
                                 reads=[("pT", pi), ("Vp", i), "Vp1"], writes=bkeys(po))
                          if i == jb:
                              for _ in range(nb_per_g):
                                  if bpos[0] < len(bsteps):
                                      bsteps[bpos[0]]()
                                      bpos[0] += 1
                              OP("act", lambda: nc.scalar.copy(out=yatt[:, g * 256:(g + 1) * 256].rearrange("p (h d) -> p h d", h=4), in_=pov[:, :, 0:64]),
                                 reads=bkeys(po), writes=["yatt"])
                              OP("act", lambda: nc.scalar.copy(out=asum[:, g * 4:(g + 1) * 4].unsqueeze(2), in_=pov[:, :, 64:65]),
                                 reads=bkeys(po), writes=["asum"])
                              if j < NJ - 1:
                                  if fill:
                                      fill.pop(0)()
                              else:
                                  while fill:
                                      fill.pop(0)()
                                  for _ in range(2):
                                      if fill2:
                                          fill2.pop(0)()

                      for idx in range(min(LA, len(its))):
                          emit_qk(idx)
                      arate = -(-len(aunits) // max(1, len(its) - 4)) if aunits else 0
                      for idx in range(len(its)):
                          if idx + LA < len(its):
                              emit_qk(idx + LA)
                          if pre is not None and idx == min(7, jb):
                              pre()
                          emit_pv(idx)
                          for _ in range(arate):
                              if aunits:
                                  aunits.pop(0)()
                      while aunits:
                          aunits.pop(0)()
                      while bpos[0] < len(bsteps):
                          bsteps[bpos[0]]()
                          bpos[0] += 1

                      def finish():
                          OP("dve", lambda: nc.vector.reciprocal(out=arec[:], in_=asum[:]), reads=["asum"], writes=["arec"])
                          OP("dve", lambda: nc.vector.tensor_tensor(
                              out=yatt[:].rearrange("p (h d) -> p h d", h=16), in0=yatt[:].rearrange("p (h d) -> p h d", h=16),
                              in1=arec[:].unsqueeze(2).broadcast_to([128, 16, 64]), op=ALU.mult),
                             reads=["yatt", "arec"], writes=["yatt"])

                          def ya_dst(s0, n_, pview, tkey):
                              OP("act", lambda: nc.scalar.copy(out=yattT[:, s0:s0 + n_, j * 128:(j + 1) * 128],
                                                               in_=pview.rearrange("p (s t) -> p s t", s=n_)),
                                 reads=[tkey], writes=["actT"])
                          transposes_to(yatt, "yatt", 8, ya_dst, None, only_bank=7)
                      return finish

                  NU = 5 * NJ
                  b0steps = None
                  a1 = None
                  for step in range(NU + 2):
                      if step < NU:
                          u_S1(step)
                      if 0 <= step - 1 < NU:
                          u_S2(step - 1)
                      if 0 <= step - 2 < NU:
                          u_S3(step - 2)
                      if step == 2 * NJ + 2:
                          for f_ in stage_A_units(0):
                              f_()
                          b0steps = stage_B_steps(0)
                      elif b0steps is not None:
                          if b0steps:
                              b0steps.pop(0)()
                          if step == 2 * NJ + 3:
                              a1 = stage_A_units(1)
                          if a1:
                              for _ in range(4):
                                  if a1:
                                      a1.pop(0)()
                  k_ = 0
                  while b0steps:
                      b0steps.pop(0)()
                      k_ += 1
                      for _ in range(4):
                          if a1:
                              a1.pop(0)()
                      if k_ % 2 == 1 and fillers:
                          fillers.pop(0)()
                  while a1:
                      a1.pop(0)()
                  while fillers:
                      fillers.pop(0)()
                  fin_prev = None
                  for j in range(NJ):
                      bst = stage_B_steps(j + 1) if j + 1 < NJ else []
                      aun = stage_A_units(j + 2) if j + 2 < NJ else []
                      fin_prev = stage_C(j, bst, fillers, fillB, aun, pre=fin_prev)
                  fin_prev()
                  while fillers:
                      fillers.pop(0)()
                  while fillB:
                      fillB.pop(0)()
                  ck("p2b", [(yrT[:, 0, :], "actT"), (yrT[:, 7, :], "actT")])

                  ck("p3", [(yattT[:, 0, :], "actT"), (yattT[:, 7, :], "actT")])
                  Sx.dma("sp", x_sb[:], x[b, tok0:tok0 + T, :].rearrange("(j p) d -> p j d", p=128), "xld", writes=["x_sb"])
                  for fc in range(KC):
                      merge_unit(1, fc, next_bank)
                  wo_p = [get_piece(("Wo", "k", g * 512, (g + 1) * 512)) for g in range(2)]

                  def post_norm_residual(banks, j, gbt, gkey):
                      for hf in range(2):
                          OP("act", lambda hf=hf: nc.scalar.activation(out=m1[hf][:], in_=ps[banks[hf]][:], func=AF.Square,
                                                                       accum_out=stat[:, hf:hf + 1]),
                             reads=bkeys(banks[hf]), writes=[("m1", hf), ("stat", hf)])
                      OP("dve", lambda: nc.vector.tensor_tensor(out=stat[:, 2:3], in0=stat[:, 0:1], in1=stat[:, 1:2], op=ALU.add),
                         reads=[("stat", 0), ("stat", 1)], writes=[("stat", 2)])
                      OP("act", lambda: nc.scalar.activation(out=stat[:, 4:5], in_=stat[:, 2:3], func=AF.Sqrt, bias=epsT[:, 0:1], scale=1.0 / D),
                         reads=[("stat", 2), "epsT"], writes=["stat_b"])
                      OP("dve", lambda: nc.vector.reciprocal(out=stat[:, 8:9], in_=stat[:, 4:5]), reads=["stat_b"], writes=["stat_c"])
                      for hf in range(2):
                          OP("dve", lambda hf=hf: nc.vector.scalar_tensor_tensor(out=m1[hf][:], in0=ps[banks[hf]][:], scalar=stat[:, 8:9],
                                                                                  in1=gbt[:, hf * 512:(hf + 1) * 512], op0=ALU.mult, op1=ALU.mult),
                             reads=bkeys(banks[hf]) + ["stat_c", gkey], writes=[("m1", hf)])
                          OP("dve", lambda hf=hf: nc.vector.tensor_tensor(out=x_sb[:, j, hf * 512:(hf + 1) * 512], in0=x_sb[:, j, hf * 512:(hf + 1) * 512],
                                                                          in1=m1[hf][:], op=ALU.add),
                             reads=[("m1", hf), "x_sb"], writes=["x_sb"])

                  for j in range(NJ):
                      banks = [next_bank(), next_bank()]
                      for hf in range(2):
                          wv_, wk_ = wo_p[hf]
                          for kc in range(KC):
                              OP("pe", lambda kc=kc, hf=hf, wv_=wv_: nc.tensor.matmul(ps[banks[hf]][:], lhsT=mergedT[:, kc, j * 128:(j + 1) * 128],
                                                                                       rhs=wv_[:, kc, :], start=(kc == 0), stop=(kc == KC - 1)),
                                 reads=["xn", wk_], writes=[("ps", banks[hf])])
                      post_norm_residual(banks, j, g2b, "g2b")

                  ck("p4", [(x_sb[:].rearrange("p j d -> p (j d)"), "x_sb")])
                  for j in range(NJ):
                      OP("act", lambda j=j: nc.scalar.activation(out=xn[:, j, :], in_=x_sb[:, j, :], func=AF.Square, accum_out=stat[:, j:j + 1]),
                         reads=["x_sb"], writes=["xn", ("stat", j)])
                  OP("act", lambda: nc.scalar.activation(out=stat[:, 4:8], in_=stat[:, 0:4], func=AF.Sqrt, bias=epsT[:, 0:1], scale=1.0 / D),
                     reads=[("stat", j) for j in range(4)] + ["epsT"], writes=["stat_b"])
                  OP("dve", lambda: nc.vector.reciprocal(out=stat[:, 8:12], in_=stat[:, 4:8]), reads=["stat_b"], writes=["stat_c"])
                  for j in range(NJ):
                      OP("dve", lambda j=j: nc.vector.tensor_scalar(out=xn[:, j, :], in0=x_sb[:, j, :], scalar1=stat[:, 8 + j:9 + j], scalar2=None, op0=ALU.mult),
                         reads=["x_sb", "stat_c"], writes=["xn"])
                  for kc in range(KC):
                      tb = 6 + next_bank("tp", 2)
                      tkey = ("pst", tb)
                      pview = psb[tb][:, 0:512]
                      for j in range(NJ):
                          OP("pe", lambda j=j, kc=kc, pview=pview: nc.tensor.transpose(
                              out=pview[:, j * 128:(j + 1) * 128], in_=xn[:, j, kc * 128:(kc + 1) * 128], identity=ident[:]),
                             reads=["xn", "ident"], writes=[tkey])
                      OP("act", lambda kc=kc, pview=pview: nc.scalar.activation(out=hT[:, kc, :], in_=pview, func=AF.Identity, scale=g3[:, kc:kc + 1]),
                         reads=[tkey, "g3"], writes=["hT"])
                  for g in range(6):
                      c0, c1 = g * 512, min(DFF, (g + 1) * 512)
                      wgv, kgv = get_piece(("Wg", "k", c0, c1))
                      wuv, kuv = get_piece(("Wu", "k", c0, c1))
                      for cl in range((c1 - c0) // 128):
                          fc = g * 4 + cl
                          bg = next_bank()
                          for kc in range(KC):
                              OP("pe", lambda kc=kc, bg=bg: nc.tensor.matmul(ps[bg][:], lhsT=wgv[:, kc, cl * 128:(cl + 1) * 128], rhs=hT[:, kc, :],
                                                                              start=(kc == 0), stop=(kc == KC - 1)),
                                 reads=["hT", kgv], writes=[("ps", bg)])
                          bu = next_bank()
                          for kc in range(KC):
                              OP("pe", lambda kc=kc, bu=bu: nc.tensor.matmul(ps[bu][:], lhsT=wuv[:, kc, cl * 128:(cl + 1) * 128], rhs=hT[:, kc, :],
                                                                              start=(kc == 0), stop=(kc == KC - 1)),
                                 reads=["hT", kuv], writes=[("ps", bu)])
                          si = next_bank("sg", 2)
                          OP("act", lambda bg=bg, si=si: nc.scalar.activation(out=sg[si][:], in_=ps[bg][:], func=AF.Silu),
                             reads=[("ps", bg)], writes=[("sg", si)])
                          OP("dve", lambda bu=bu, si=si, fc=fc: nc.vector.tensor_tensor(out=actT[:, fc, :], in0=ps[bu][:], in1=sg[si][:], op=ALU.mult),
                             reads=[("ps", bu), ("sg", si)], writes=["actT", "rrA", "iiA", "ptmpA"])
                  for g in range(6):
                      f0, f1 = g * 4, min(FC, (g + 1) * 4)
                      wdv, kdv = get_piece(("Wd", "d", f0, f1))
                      for fl in range(f1 - f0):
                          fc = f0 + fl
                          for j in range(NJ):
                              for hf in range(2):
                                  bk = j * 2 + hf
                                  OP("pe", lambda fl=fl, fc=fc, j=j, hf=hf, bk=bk: nc.tensor.matmul(
                                      ps[bk][:], lhsT=actT[:, fc, j * 128:(j + 1) * 128], rhs=wdv[:, fl, hf * 512:(hf + 1) * 512],
                                      start=(fc == 0), stop=(fc == FC - 1)),
                                     reads=["actT", "rrA", "iiA", "ptmpA", kdv], writes=bkeys(bk))
                  for j in range(NJ):
                      post_norm_residual([j * 2, j * 2 + 1], j, g4b, "g4b")
                  Sx.dma("sp", out[b, tok0:tok0 + T, :].rearrange("(j p) d -> p j d", p=128), x_sb[:], "ost", reads=["x_sb"])

        except _Stop:
            pass
        if "ost" in Sx.dsem:
            nc.sync.wait_ge(Sx.dsem["ost"], Sx.dcnt["ost"])
        if "dbg" in Sx.dsem:
            nc.sync.wait_ge(Sx.dsem["dbg"], Sx.dcnt["dbg"])
        stats = dict(ninstr=Sx.ninstr, nwaits=Sx.nwaits, nsem=len(Sx.dsem) + 4)
    return nc, stats


def _consts(S):
    half = 8
    inv_freq = (500000.0 ** (-np.arange(half, dtype=np.float32) / half)).astype(np.float32)
    ang = (np.arange(S, dtype=np.float32)[:, None] * inv_freq[None, :]).astype(np.float32)
    cos = np.cos(ang).astype(np.float32)
    sin = np.sin(ang).astype(np.float32)
    rope = np.concatenate([cos, cos, -sin, sin], axis=1).astype(np.float32)
    ident = np.eye(128, dtype=np.float32)
    pow2 = np.tile((2.0 ** -(np.arange(64, dtype=np.float32) + 1.0))[None, :], (128, 1)).astype(np.float32)
    return rope, ident, pow2


_CACHE = {}


def kernel(**inputs):
    x = np.ascontiguousarray(inputs["x"], dtype=np.float32)
    B, S, _ = x.shape
    ncores = 8
    NB = B // ncores
    key = (NB, S)
    if key not in _CACHE:
        _CACHE[key] = build_program(NB, S)
    nc, stats = _CACHE[key]
    rope, ident, pow2 = _consts(S)
    in_maps = []
    for c in range(ncores):
        m = {k: np.ascontiguousarray(v, dtype=np.float32) for k, v in inputs.items() if k != "x"}
        m["x"] = x[c * NB:(c + 1) * NB]
        m["rope_tab"] = rope
        m["ident_in"] = ident
        m["pow2_in"] = pow2
        in_maps.append(m)
    res = run_bass_kernel_spmd(nc, in_maps, core_ids=list(range(ncores)))
    return np.concatenate([np.asarray(r["out"]) for r in res.results], axis=0).astype(np.float32)
```
